# Optimizing a Trainium2 kernel written in Bass

```python
import jax, jax.numpy as jnp
from jax import lax
import numpy as np

D_MODEL = 1024
BATCH = 4
SEQ = 8192
DEPTH = 2

GRID_W = 64
CTX_LEN = 256
N_MIXERS = 2
EPS = 1e-6
MLA_HEADS = 8
MLA_Q_RANK = 512
MLA_KV_RANK = 256
MLA_NOPE = 128
MLA_ROPE = 64
MLA_V = 128
Q_BLOCK = 128
ROPE_BASE = 10000.0
GLA_HEADS = 4
GLA_DK = D_MODEL // 2 // GLA_HEADS
GLA_DV = D_MODEL // GLA_HEADS
GLA_GATE_RANK = 16
GLA_GATE_NORM = 16.0
GLA_CHUNK = 64
N_EXPERTS = 32
TOP_K = 4
D_FF = D_MODEL
SWIGLU_ALPHA = 1.702
SWIGLU_LIMIT = 7.0
MOE_BLOCK = 128

kernel_name = 'hybrid_mla_gla_moe_dit_trunk'


def rmsnorm(x, g):
    xf = x.astype(jnp.float32)
    y = xf * lax.rsqrt(jnp.mean(xf * xf, axis=-1, keepdims=True) + EPS)
    return (y * g.astype(jnp.float32)).astype(x.dtype)


def axial_rope_tables(n):
    rows = n // GRID_W
    row = jnp.repeat(jnp.arange(rows, dtype=jnp.float32), GRID_W)
    col = jnp.tile(jnp.arange(GRID_W, dtype=jnp.float32), rows)
    nf = MLA_ROPE // 4
    inv = jnp.power(ROPE_BASE, -jnp.arange(nf, dtype=jnp.float32) / nf)
    ang = jnp.concatenate([row[:, None] * inv, col[:, None] * inv], axis=-1)
    return jnp.cos(ang), jnp.sin(ang)


def apply_axial_rope(x, cos, sin):
    nf = MLA_ROPE // 4
    shp = (cos.shape[0],) + (1,) * (x.ndim - 3) + (2, nf)
    c = cos.reshape(shp)
    s = sin.reshape(shp)
    xs = x.astype(jnp.float32).reshape(x.shape[:-1] + (2, 2, nf))
    x1, x2 = xs[..., 0, :], xs[..., 1, :]
    out = jnp.stack([x1 * c - x2 * s, x2 * c + x1 * s], axis=-2)
    return out.reshape(x.shape).astype(x.dtype)


def mla_mixer(h, hc, cos, sin, w_in, q_norm, w_uq, kv_norm, w_ukv, w_o, with_ctx_out):
    H = MLA_HEADS
    w_uk = w_ukv[:, :H * MLA_NOPE].reshape(MLA_KV_RANK, H, MLA_NOPE)
    w_uv = w_ukv[:, H * MLA_NOPE:].reshape(MLA_KV_RANK, H, MLA_V)
    scale = (MLA_NOPE + MLA_ROPE) ** -0.5

    def project(z, rotate):
        b, l, _ = z.shape
        a = z @ w_in
        cq = rmsnorm(a[..., :MLA_Q_RANK], q_norm)
        ckv = rmsnorm(a[..., MLA_Q_RANK:MLA_Q_RANK + MLA_KV_RANK], kv_norm)
        kr = a[..., MLA_Q_RANK + MLA_KV_RANK:]
        q = (cq @ w_uq).reshape(b, l, H, MLA_NOPE + MLA_ROPE)
        q_nope, q_rope = q[..., :MLA_NOPE], q[..., MLA_NOPE:]
        if rotate:
            q_rope = apply_axial_rope(q_rope, cos, sin)
            kr = apply_axial_rope(kr, cos, sin)
        q_eff = jnp.concatenate([jnp.einsum('blhn,chn->blhc', q_nope, w_uk), q_rope], axis=-1)
        keys = jnp.concatenate([ckv, kr], axis=-1)
        return q_eff, keys, ckv

    def attend(q, keys, vals):
        s = jnp.einsum('bqhc,bkc->bhqk', q, keys).astype(jnp.float32) * scale
        p = jax.nn.softmax(s, axis=-1).astype(vals.dtype)
        return jnp.einsum('bhqk,bkc->bqhc', p, vals)

    def out_proj(o):
        b, l = o.shape[:2]
        return jnp.einsum('blhc,chv->blhv', o, w_uv).reshape(b, l, H * MLA_V) @ w_o

    q_l, k_l, v_l = project(h, True)
    q_c, k_c, v_c = project(hc, False)
    k_all = jnp.concatenate([k_l, k_c], axis=1)
    v_all = jnp.concatenate([v_l, v_c], axis=1)
    B, N = h.shape[:2]
    nb = N // Q_BLOCK
    q_blocks = q_l.reshape(B, nb, Q_BLOCK, H, MLA_KV_RANK + MLA_ROPE).transpose(1, 0, 2, 3, 4)
    o = lax.map(lambda qb: attend(qb, k_all, v_all), q_blocks)
    o = o.transpose(1, 0, 2, 3, 4).reshape(B, N, H, MLA_KV_RANK)
    y = out_proj(o)
    yc = out_proj(attend(q_c, k_c, v_c)) if with_ctx_out else None
    return y, yc


def gla_scan(q, k, v, g, s0):
    B, L, H, DK = q.shape
    DV = v.shape[-1]
    C = GLA_CHUNK
    nc = L // C
    mask = jnp.tril(jnp.ones((C, C), dtype=bool))

    def chunks(t):
        return t.reshape(B, nc, C, H, t.shape[-1]).transpose(1, 0, 3, 2, 4).astype(jnp.float32)

    def step(S, inp):
        qc, kc, vc, gc = inp
        b = jnp.cumsum(gc, axis=2)
        o_inter = jnp.einsum('bhcd,bhde->bhce', qc * jnp.exp(b), S)
        diff = b[:, :, :, None, :] - b[:, :, None, :, :]
        decay = jnp.exp(jnp.where(mask[:, :, None], diff, -jnp.inf))
        A = jnp.einsum('bhid,bhijd,bhjd->bhij', qc, decay, kc)
        o_intra = jnp.einsum('bhij,bhje->bhie', A, vc)
        b_last = b[:, :, -1:, :]
        S_new = jnp.exp(b_last[:, :, 0, :])[..., None] * S + jnp.einsum('bhjd,bhje->bhde', kc * jnp.exp(b_last - b), vc)
        return S_new, o_inter + o_intra

    s_fin, o = lax.scan(step, s0, (chunks(q), chunks(k), chunks(v), chunks(g)))
    o = o.transpose(1, 0, 3, 2, 4).reshape(B, L, H, DV).astype(v.dtype)
    return o, s_fin


def gla_mixer(h, hc, w_in, w_gate1, w_gate2, b_gate, head_norm, w_o, with_ctx_out):
    H = GLA_HEADS
    nqk = H * GLA_DK
    nv = H * GLA_DV

    def project(z):
        b, l, _ = z.shape
        a = z @ w_in
        q = a[..., :nqk].reshape(b, l, H, GLA_DK) * (GLA_DK ** -0.5)
        k = a[..., nqk:2 * nqk].reshape(b, l, H, GLA_DK)
        v = a[..., 2 * nqk:2 * nqk + nv].reshape(b, l, H, GLA_DV)
        r = a[..., 2 * nqk + nv:]
        g_f = (jax.nn.log_sigmoid((z @ w_gate1[0]) @ w_gate2[0] + b_gate[0]) / GLA_GATE_NORM).reshape(b, l, H, GLA_DK)
        g_b = (jax.nn.log_sigmoid((z @ w_gate1[1]) @ w_gate2[1] + b_gate[1]) / GLA_GATE_NORM).reshape(b, l, H, GLA_DK)
        return q, k, v, r, g_f, g_b

    def flip(t):
        return jnp.flip(t, axis=1)

    def finish(o, r):
        b, l = o.shape[:2]
        o = rmsnorm(o, head_norm).reshape(b, l, nv)
        return (o * jax.nn.silu(r)) @ w_o

    B = h.shape[0]
    s0 = jnp.zeros((B, H, GLA_DK, GLA_DV), jnp.float32)
    qc, kc, vc, rc, gcf, gcb = project(hc)
    oc_f, sc_f = gla_scan(qc, kc, vc, gcf, s0)
    oc_b, sc_b = gla_scan(flip(qc), flip(kc), flip(vc), flip(gcb), s0)
    q, k, v, r, g_f, g_b = project(h)
    o_f, _ = gla_scan(q, k, v, g_f, sc_f)
    o_b, _ = gla_scan(flip(q), flip(k), flip(v), flip(g_b), sc_b)
    y = finish(o_f + flip(o_b), r)
    yc = finish(oc_f + flip(oc_b), rc) if with_ctx_out else None
    return y, yc


def moe(t, w_router, b_router, w_gu, b_gu, w_down, b_down):
    T, D = t.shape
    logits = (t @ w_router + b_router).astype(jnp.float32)
    top_val, top_idx = lax.top_k(logits, TOP_K)
    gate = jax.nn.softmax(top_val, axis=-1)
    n_assign = T * TOP_K
    flat_e = top_idx.reshape(-1)
    flat_tok = jnp.repeat(jnp.arange(T, dtype=jnp.int32), TOP_K)
    flat_w = gate.reshape(-1)
    order = jnp.argsort(flat_e)
    se = flat_e[order]
    counts = jnp.bincount(flat_e, length=N_EXPERTS)
    padded = (counts + MOE_BLOCK - 1) // MOE_BLOCK * MOE_BLOCK
    start_s = jnp.cumsum(counts) - counts
    end_p = jnp.cumsum(padded)
    start_p = end_p - padded
    dest = start_p[se] + jnp.arange(n_assign) - start_s[se]
    n_rows = (n_assign + MOE_BLOCK - 1) // MOE_BLOCK * MOE_BLOCK + N_EXPERTS * MOE_BLOCK
    n_blocks = n_rows // MOE_BLOCK
    row_tok = jnp.zeros((n_rows,), jnp.int32).at[dest].set(flat_tok[order])
    row_w = jnp.zeros((n_rows,), jnp.float32).at[dest].set(flat_w[order])
    block_e = jnp.minimum(jnp.searchsorted(end_p, jnp.arange(n_blocks) * MOE_BLOCK, side='right'), N_EXPERTS - 1)

    def block_fn(args):
        tok, w, e = args
        xb = t[tok]
        gu = xb @ w_gu[e] + b_gu[e]
        g_, u_ = gu[:, ::2], gu[:, 1::2]
        g_ = jnp.minimum(g_, SWIGLU_LIMIT)
        u_ = jnp.clip(u_, -SWIGLU_LIMIT, SWIGLU_LIMIT)
        y = (u_ + 1) * (g_ * jax.nn.sigmoid(SWIGLU_ALPHA * g_))
        y = y @ w_down[e] + b_down[e]
        return y * w[:, None].astype(y.dtype)

    rows = lax.map(block_fn, (row_tok.reshape(n_blocks, MOE_BLOCK), row_w.reshape(n_blocks, MOE_BLOCK), block_e))
    return jax.ops.segment_sum(rows.reshape(n_rows, D), row_tok, num_segments=T)


def setup_inputs(seed: int = 0) -> dict:
    key = jax.random.key(seed)
    ks = jax.random.split(key, 32)
    D = D_MODEL
    n_mla = (DEPTH + 1) // 2
    n_gla = DEPTH // 2
    mla_in = MLA_Q_RANK + MLA_KV_RANK + MLA_ROPE
    gla_in = 2 * GLA_HEADS * GLA_DK + 2 * GLA_HEADS * GLA_DV

    def nrm(k, shape, scale):
        return jax.random.normal(k, shape, jnp.float32) * scale

    return {
        'x': nrm(ks[0], (BATCH, SEQ, D), 1.0),
        'c': nrm(ks[1], (BATCH, D), 1.0),
        'ctx': nrm(ks[2], (BATCH, CTX_LEN, D), 1.0),
        'c_ctx': nrm(ks[3], (D,), 1.0),
        'mod_w': nrm(ks[4], (DEPTH, D, 6 * D), 0.3 * D ** -0.5),
        'mod_b': nrm(ks[5], (DEPTH, 6 * D), 0.02),
        'norm1': 1.0 + nrm(ks[6], (DEPTH, D), 0.05),
        'norm2': 1.0 + nrm(ks[7], (DEPTH, D), 0.05),
        'mla_w_in': nrm(ks[8], (n_mla, D, mla_in), D ** -0.5),
        'mla_q_norm': 1.0 + nrm(ks[9], (n_mla, MLA_Q_RANK), 0.05),
        'mla_w_uq': nrm(ks[10], (n_mla, MLA_Q_RANK, MLA_HEADS * (MLA_NOPE + MLA_ROPE)), MLA_Q_RANK ** -0.5),
        'mla_kv_norm': 1.0 + nrm(ks[11], (n_mla, MLA_KV_RANK), 0.05),
        'mla_w_ukv': nrm(ks[12], (n_mla, MLA_KV_RANK, MLA_HEADS * (MLA_NOPE + MLA_V)), MLA_KV_RANK ** -0.5),
        'mla_w_o': nrm(ks[13], (n_mla, MLA_HEADS * MLA_V, D), (MLA_HEADS * MLA_V) ** -0.5),
        'gla_w_in': nrm(ks[14], (n_gla, D, gla_in), D ** -0.5),
        'gla_w_gate1': nrm(ks[15], (n_gla, 2, D, GLA_GATE_RANK), D ** -0.5),
        'gla_w_gate2': nrm(ks[16], (n_gla, 2, GLA_GATE_RANK, GLA_HEADS * GLA_DK), GLA_GATE_RANK ** -0.5),
        'gla_b_gate': nrm(ks[17], (n_gla, 2, GLA_HEADS * GLA_DK), 0.1),
        'gla_head_norm': 1.0 + nrm(ks[18], (n_gla, GLA_DV), 0.05),
        'gla_w_o': nrm(ks[19], (n_gla, GLA_HEADS * GLA_DV, D), (GLA_HEADS * GLA_DV) ** -0.5),
        'moe_w_router': nrm(ks[20], (DEPTH, D, N_EXPERTS), D ** -0.5),
        'moe_b_router': nrm(ks[21], (DEPTH, N_EXPERTS), 0.01),
        'moe_w_gu': nrm(ks[22], (DEPTH, N_EXPERTS, D, 2 * D_FF), D ** -0.5),
        'moe_b_gu': nrm(ks[23], (DEPTH, N_EXPERTS, 2 * D_FF), 0.02),
        'moe_w_down': nrm(ks[24], (DEPTH, N_EXPERTS, D_FF, D), D_FF ** -0.5),
        'moe_b_down': nrm(ks[25], (DEPTH, N_EXPERTS, D), 0.02),
        'final_norm': 1.0 + nrm(ks[26], (D,), 0.05),
    }


def reference(x, c, ctx, c_ctx, mod_w, mod_b, norm1, norm2, mla_w_in, mla_q_norm, mla_w_uq, mla_kv_norm, mla_w_ukv, mla_w_o,
              gla_w_in, gla_w_gate1, gla_w_gate2, gla_b_gate, gla_head_norm, gla_w_o,
              moe_w_router, moe_b_router, moe_w_gu, moe_b_gu, moe_w_down, moe_b_down, final_norm):
    B, N, D = x.shape
    Lc = ctx.shape[1]
    cos, sin = axial_rope_tables(N)
    xc = ctx
    for i in range(DEPTH):
        last = i == DEPTH - 1
        mod = (jax.nn.silu(c) @ mod_w[i] + mod_b[i])[:, None, :]
        mod_c = (jax.nn.silu(c_ctx) @ mod_w[i] + mod_b[i])[None, None, :]
        sh1, sc1, g1, sh2, sc2, g2 = jnp.split(mod, 6, axis=-1)
        sh1c, sc1c, g1c, sh2c, sc2c, g2c = jnp.split(mod_c, 6, axis=-1)
        h = rmsnorm(x, norm1[i]) * (1 + sc1) + sh1
        hc = rmsnorm(xc, norm1[i]) * (1 + sc1c) + sh1c
        j = i // N_MIXERS
        if i % N_MIXERS == 0:
            y, yc = mla_mixer(h, hc, cos, sin, mla_w_in[j], mla_q_norm[j], mla_w_uq[j], mla_kv_norm[j], mla_w_ukv[j], mla_w_o[j], not last)
        else:
            y, yc = gla_mixer(h, hc, gla_w_in[j], gla_w_gate1[j], gla_w_gate2[j], gla_b_gate[j], gla_head_norm[j], gla_w_o[j], not last)
        x = x + g1 * y
        h = rmsnorm(x, norm2[i]) * (1 + sc2) + sh2
        if last:
            y = moe(h.reshape(B * N, D), moe_w_router[i], moe_b_router[i], moe_w_gu[i], moe_b_gu[i], moe_w_down[i], moe_b_down[i]).reshape(B, N, D)
            x = x + g2 * y
        else:
            xc = xc + g1c * yc
            hc = rmsnorm(xc, norm2[i]) * (1 + sc2c) + sh2c
            tokens = jnp.concatenate([h.reshape(B * N, D), hc.reshape(B * Lc, D)], axis=0)
            out = moe(tokens, moe_w_router[i], moe_b_router[i], moe_w_gu[i], moe_b_gu[i], moe_w_down[i], moe_b_down[i])
            x = x + g2 * out[:B * N].reshape(B, N, D)
            xc = xc + g2c * out[B * N:].reshape(B, Lc, D)
    return rmsnorm(x, final_norm)
```

```python
import numpy as np
from contextlib import ExitStack
import ml_dtypes
import concourse.bass as bass
import concourse.mybir as mybir
from concourse.bass_utils import run_bass_kernel_spmd

F32 = mybir.dt.float32
BF16 = mybir.dt.bfloat16
I32 = mybir.dt.int32
AF = mybir.ActivationFunctionType
ALU = mybir.AluOpType

D = 1024
B = 4
N = 8192
LC = 256
NOWN = 4096
NKEY = N + LC
NT_OWN = NOWN // 128
NT_KEY = NKEY // 128
H = 8
EPS = 1e-6
SCALE = (128 + 64) ** -0.5

ENGS = ['pe', 'act', 'dve', 'pool', 'sp']
SAME_ENGINE_SYNC = {'pe': False, 'act': True, 'dve': True, 'pool': True, 'sp': False}


class Res:
    __slots__ = ('name', 'w', 'rs')

    def __init__(self, name=''):
        self.name = name
        self.w = None
        self.rs = []


class T:
    __slots__ = ('t', 'r')

    def __init__(self, t, r):
        self.t = t
        self.r = r

    def __getitem__(self, k):
        return self.t[k]


class Prog:
    def __init__(self, nc, stack, n_dma_sems=8):
        self.nc = nc
        self.stack = stack
        self.q = {e: [] for e in ENGS}
        self.cnt = {e: 0 for e in ENGS}
        self.esem = {e: stack.enter_context(nc.semaphore('es_' + e)) for e in ENGS if e != 'sp'}
        self.dq = {}
        for qn in ('sp', 'pool', 'act'):
            n = n_dma_sems
            self.dq[qn] = dict(sems=[stack.enter_context(nc.semaphore(f'ds_{qn}{i}')) for i in range(n)],
                               cnt=[0] * n, nxt=0)
        self.seen = {e: {} for e in ENGS}
        self.n_wait = 0
        self.n_dma = 0

    def _uniq(self, name):
        self._n = getattr(self, "_n", 0) + 1
        return f"{name}_{self._n}"

    def sb(self, name, shape, dtype, stack=None):
        t = (stack or self.stack).enter_context(self.nc.sbuf_tensor(self._uniq("sb_" + name), list(shape), dtype))
        return T(t, Res(name))

    def ps(self, name, shape, dtype=F32, stack=None):
        t = (stack or self.stack).enter_context(self.nc.psum_tensor(self._uniq("ps_" + name), list(shape), dtype))
        return T(t, Res(name))

    def dram(self, name, shape, dtype, kind="Internal"):
        t = self.nc.dram_tensor(self._uniq("dr_" + name), list(shape), dtype, kind=kind)
        return T(t.ap(), Res(name))

    def _deps(self, reads, writes):
        evs = []
        for r in reads:
            r = r.r if isinstance(r, T) else r
            if r.w is not None:
                evs.append(r.w)
        for w in writes:
            w = w.r if isinstance(w, T) else w
            if w.w is not None:
                evs.append(w.w)
            evs.extend(w.rs)
        return evs

    def _waits(self, eng, evs):
        out = []
        seen = self.seen[eng]
        best = {}
        for ev in evs:
            kind, key, val = ev
            if kind == 'e' and key == eng and not SAME_ENGINE_SYNC[eng]:
                continue
            k = (kind, key)
            if best.get(k, 0) < val:
                best[k] = val
        for k, val in best.items():
            if seen.get(k, 0) >= val:
                continue
            seen[k] = val
            sem = self.esem[k[1]] if k[0] == 'e' else self.dq[k[1][0]]['sems'][k[1][1]]
            out.append((sem, val))
        return out

    def _commit(self, ev, reads, writes):
        for r in reads:
            r = r.r if isinstance(r, T) else r
            r.rs.append(ev)
            if len(r.rs) > 64:
                best = {}
                for e in r.rs:
                    k = (e[0], e[1])
                    if best.get(k, (0, 0, 0))[2] < e[2]:
                        best[k] = e
                r.rs = list(best.values())
        for w in writes:
            w = w.r if isinstance(w, T) else w
            w.w = ev
            w.rs = []

    def op(self, eng, fn, reads=(), writes=()):
        waits = self._waits(eng, self._deps(reads, writes))
        self.cnt[eng] += 1
        ev = ('e', eng, self.cnt[eng])
        self.q[eng].append((waits, fn, (self.esem[eng], 1)))
        self.n_wait += len(waits)
        self._commit(ev, reads, writes)
        return ev

    def dma(self, qn, fn, reads=(), writes=()):
        dq = self.dq[qn]
        i = dq['nxt']
        dq['nxt'] = (i + 1) % len(dq['sems'])
        evs = self._deps(reads, writes)
        if dq['cnt'][i] > 0:
            evs.append(('d', (qn, i), dq['cnt'][i]))
        waits = self._waits(qn, evs)
        dq['cnt'][i] += 16
        ev = ('d', (qn, i), dq['cnt'][i])
        self.q[qn].append((waits, fn, (dq['sems'][i], 16)))
        self.n_wait += len(waits)
        self.n_dma += 1
        self._commit(ev, reads, writes)
        return ev

    def barrier(self):
        evs = []
        for qn, dq in self.dq.items():
            for i, c in enumerate(dq['cnt']):
                if c:
                    evs.append(('d', (qn, i), c))
        for e in ('pe', 'act', 'dve', 'pool'):
            if self.cnt[e]:
                evs.append(('e', e, self.cnt[e]))
        for eng in ENGS:
            waits = self._waits(eng, evs)
            if waits:
                self.q[eng].append((waits, None, None))
                self.n_wait += len(waits)

    def finish(self):
        evs = []
        for qn, dq in self.dq.items():
            for i, c in enumerate(dq['cnt']):
                if c:
                    evs.append(('d', (qn, i), c))
        for e in ('pe', 'act', 'dve', 'pool'):
            if self.cnt[e]:
                evs.append(('e', e, self.cnt[e]))
        waits = self._waits('sp', evs)
        self.q['sp'].append((waits, None, None))

    def emit(self):
        nc = self.nc
        q = self.q

        def run(engine, name):
            for waits, fn, inc in q[name]:
                for sem, val in waits:
                    engine.wait_ge(sem, val)
                if fn is not None:
                    ins = fn(engine)
                    ins.then_inc(inc[0], inc[1])

        with nc.Block() as block:
            @block.tensor
            def _(e):
                run(e, 'pe')

            @block.scalar
            def _(e):
                run(e, 'act')

            @block.vector
            def _(e):
                run(e, 'dve')

            @block.gpsimd
            def _(e):
                run(e, 'pool')

            @block.sync
            def _(e):
                run(e, 'sp')


def build_program(stage_limit=99):
    nc = bass.Bass("TRN2", target_bir_lowering=False)

    def din(name, shape, dt=F32):
        return nc.dram_tensor(name, list(shape), dt, kind="ExternalInput").ap()

    xk = din("xk", [NKEY, D])
    cvec = din("cvec", [128, 8, 2])
    ident_d = din("ident", [128, 128])
    mod_w = din("mod_w", [2, D, 6 * D])
    mod_b = din("mod_b", [2, 1, 6 * D])
    norm1 = din("norm1", [2, 1, D])
    norm2 = din("norm2", [2, 1, D])
    w_in = din("mla_w_in", [D, 832])
    w_in_sw = din("mla_w_kr_sw", [D, 64])
    q_norm = din("mla_q_norm", [1, 512])
    kv_norm = din("mla_kv_norm", [1, 256])
    w_uq_n = din("w_uq_nope", [512, H * 128])
    w_uq_r = din("w_uq_rope", [512, H * 64])
    w_uq_rs = din("w_uq_rope_sw", [512, H * 64])
    w_uk = din("w_uk", [256, H * 128])
    w_uv = din("w_uv", [256, H * 128])
    w_o = din("mla_w_o", [D, D])
    ropeT_k = din("ropeT_k", [64, 2, NKEY])
    m_wr = din("moe_w_router", [D, NE])
    m_br = din("moe_b_router", [1, NE])
    m_wgu = din("moe_w_gu", [NE, D, 2 * D])
    m_bgu = din("moe_b_gu", [NE, 2 * D])
    m_wd = din("moe_w_down", [NE, D, D])
    m_bd = din("moe_b_down", [NE, D])
    out = nc.dram_tensor("out", [NOWN, D], F32, kind="ExternalOutput").ap()
    outc = nc.dram_tensor("outc", [LC, D], F32, kind="ExternalOutput").ap()
    dbg_o = [nc.dram_tensor("dbg", [2048, 4096], BF16, kind="ExternalOutput").ap()] if stage_limit <= 2 else None

    with ExitStack() as st:
        P = Prog(nc, st)
        KTn = P.dram("KTn", [H, 128, NKEY], BF16)
        Vd = P.dram("Vd", [NKEY, H * 128], BF16)
        QTn = P.dram("QTn", [H, 128, NOWN], BF16)
        QTr = P.dram("QTr", [H, 64, NOWN], BF16)

        ident = P.sb("ident", [128, 128], F32)
        identb = P.sb("identb", [128, 128], BF16)
        ones_f = P.sb("ones_f", [128, 128], F32)
        epsc = P.sb("epsc", [128, 1], F32)
        P.dma('sp', lambda e: e.dma_start(out=ident[:], in_=ident_d[:, :]), writes=[ident])
        P.op('dve', lambda e: e.tensor_copy(out=identb[:], in_=ident[:]), reads=[ident], writes=[identb])
        P.op('pool', lambda e: e.memset(ones_f[:], 1.0), writes=[ones_f])
        P.op('pool', lambda e: e.memset(epsc[:], EPS), writes=[epsc])

        g1_bc = P.sb("g1_bc", [128, D], F32)
        g1c_bc = P.sb("g1c_bc", [128, D], F32)
        X1_d = P.dram("X1_d", [NOWN + LC, D], F32)
        bc = [[P.sb(f"bc{j}_{v}", [128, D], F32) for v in range(2)] for j in range(2)]
        modrow_d = P.dram("modrow_d", [2, 6 * D], F32)
        with ExitStack() as s1:
            cv = P.sb("cv", [128, 8, 2], F32, s1)
            scv = P.sb("scv", [128, 8, 2], F32, s1)
            modrow = [P.sb(f"modrow{j}", [1, 6 * D], F32, s1) for j in range(2)]
            mb = P.sb("mb", [1, 6 * D], F32, s1)
            n1 = P.sb("n1", [1, D], F32, s1)
            n2 = P.sb("n2", [1, D], F32, s1)
            wblk = [P.sb(f"wblk{i}", [128, 8, 512], F32, s1) for i in range(2)]
            mps = [P.ps(f"mps{j}", [1, 512], F32, s1) for j in range(2)]
            bps = [P.ps(f"bps{i}", [128, 512], F32, s1) for i in range(2)]
            P.dma('sp', lambda e: e.dma_start(out=cv[:], in_=cvec[:, :, :]), writes=[cv])
            P.dma('sp', lambda e: e.dma_start(out=mb[:], in_=mod_b[0]), writes=[mb])
            P.dma('sp', lambda e: e.dma_start(out=n1[:], in_=norm1[0]), writes=[n1])
            P.dma('sp', lambda e: e.dma_start(out=n2[:], in_=norm2[0]), writes=[n2])
            P.op('act', lambda e: e.activation(out=scv[:], in_=cv[:], func=AF.Silu), reads=[cv], writes=[scv])
            mw = mod_w[0].rearrange("(k p) n -> p k n", p=128)
            for cb in range(12):
                wb_ = wblk[cb % 2]
                P.dma('sp' if cb % 2 == 0 else 'pool',
                      lambda e, wb_=wb_, cb=cb: e.dma_start(out=wb_[:], in_=mw[:, :, cb * 512:(cb + 1) * 512]),
                      writes=[wb_])
                for j in range(2):
                    for k in range(8):
                        P.op('pe', lambda e, j=j, k=k, wb_=wb_: e.matmul(mps[j][:], lhsT=scv[:, k, j:j + 1], rhs=wb_[:, k, :],
                                                                        start=(k == 0), stop=(k == 7)),
                             reads=[scv, wb_], writes=[mps[j]])
                    P.op('dve', lambda e, j=j, cb=cb: e.tensor_tensor(out=modrow[j][:, cb * 512:(cb + 1) * 512], in0=mps[j][:],
                                                                     in1=mb[:, cb * 512:(cb + 1) * 512], op=ALU.add),
                         reads=[mps[j], mb], writes=[modrow[j]])
            for j in range(2):
                for (lo, nrm) in ((D, n1), (4 * D, n2)):
                    P.op('dve', lambda e, j=j, lo=lo, nrm=nrm: e.scalar_tensor_tensor(
                        out=modrow[j][:, lo:lo + D], in0=modrow[j][:, lo:lo + D], scalar=1.0, in1=nrm[:],
                        op0=ALU.add, op1=ALU.mult), reads=[modrow[j], nrm], writes=[modrow[j]])
            seg = {0: 1, 1: 0, 2: 2, 3: 4, 4: 3, 5: 5}
            i = 0
            for j in range(2):
                P.dma('sp', lambda e, j=j: e.dma_start(out=modrow_d[j:j + 1, :], in_=modrow[j][:]), reads=[modrow[j]], writes=[modrow_d])
                for v in range(3):
                    dst = (g1_bc if j == 0 else g1c_bc) if v == 2 else bc[j][v]
                    for hf in range(2):
                        lo = seg[v] * D + hf * 512
                        bp = bps[i % 2]
                        P.op('pe', lambda e, bp=bp, j=j, lo=lo: e.matmul(bp[:], lhsT=ones_f[0:1, :], rhs=modrow[j][:, lo:lo + 512],
                                                                       start=True, stop=True),
                             reads=[ones_f, modrow[j]], writes=[bp])
                        P.op('act', lambda e, bp=bp, dst=dst, hf=hf: e.activation(out=dst[:, hf * 512:(hf + 1) * 512],
                                                                                in_=bp[:], func=AF.Copy),
                             reads=[bp], writes=[dst])
                        i += 1

        P.barrier()
        att = ExitStack()
        st.enter_context(att)
        ckvT = P.sb("ckvT", [128, 2, NKEY], BF16, att)
        krT = P.sb("krT", [64, NKEY], BF16, att)
        cqT = P.sb("cqT", [128, 4, NOWN + 512], BF16, att)
        P.op('pool', lambda e: e.memset(cqT[:, :, NOWN + LC:NOWN + 512], 0.0), writes=[cqT])
        w_uqn_b = P.sb("w_uqn_b", [128, 4, H * 128], BF16, att)
        w_uqr_b = P.sb("w_uqr_b", [128, 4, H * 64], BF16, att)
        w_uqs_b = P.sb("w_uqs_b", [128, 4, H * 64], BF16, att)
        w_uk_b = P.sb("w_uk_b", [128, 2, H * 128], BF16, att)
        w_uv_b = P.sb("w_uv_b", [128, 2, H * 128], BF16, att)
        with ExitStack() as s2:
            w_in_b = P.sb("w_in_b", [128, 8, 832], BF16, s2)
            w_sw_b = P.sb("w_sw_b", [128, 8, 64], BF16, s2)
            with ExitStack() as s2w:
                stg = P.sb("wstg", [128, 8 * 832], F32, s2w)

                def load_bf(dst, src2d, kc, ncol, q='sp'):
                    v = stg[:, 0:kc * ncol]
                    v3 = v.rearrange("p (k n) -> p k n", k=kc)
                    P.dma(q, lambda e: e.dma_start(out=v3, in_=src2d.rearrange("(k p) n -> p k n", p=128)), writes=[stg])
                    P.op('pool', lambda e: e.tensor_copy(out=dst[:].rearrange("p k n -> p (k n)"), in_=v), reads=[stg], writes=[dst])

                load_bf(w_in_b, w_in, 8, 832)
                load_bf(w_sw_b, w_in_sw, 8, 64)
                load_bf(w_uqn_b, w_uq_n, 4, H * 128)
                load_bf(w_uqr_b, w_uq_r, 4, H * 64)
                load_bf(w_uqs_b, w_uq_rs, 4, H * 64)
                load_bf(w_uk_b, w_uk, 2, H * 128)
                load_bf(w_uv_b, w_uv, 2, H * 128)
            P.barrier()
            qn_bc = P.sb("qn_bc", [128, 512], F32, s2)
            kvn_bc = P.sb("kvn_bc", [128, 256], F32, s2)
            with ExitStack() as s2n:
                qn_r = P.sb("qn_r", [1, 512], F32, s2n)
                kvn_r = P.sb("kvn_r", [1, 256], F32, s2n)
                nps = P.ps("nps", [128, 512], F32, s2n)
                P.dma('sp', lambda e: e.dma_start(out=qn_r[:], in_=q_norm[:, :]), writes=[qn_r])
                P.dma('sp', lambda e: e.dma_start(out=kvn_r[:], in_=kv_norm[:, :]), writes=[kvn_r])
                P.op('pe', lambda e: e.matmul(nps[:], lhsT=ones_f[0:1, :], rhs=qn_r[:], start=True, stop=True),
                     reads=[ones_f, qn_r], writes=[nps])
                P.op('act', lambda e: e.activation(out=qn_bc[:], in_=nps[:], func=AF.Copy), reads=[nps], writes=[qn_bc])
                P.op('pe', lambda e: e.matmul(nps[:, 0:256], lhsT=ones_f[0:1, :], rhs=kvn_r[:], start=True, stop=True),
                     reads=[ones_f, kvn_r, qn_bc], writes=[nps])
                P.op('act', lambda e: e.activation(out=kvn_bc[:], in_=nps[:, 0:256], func=AF.Copy), reads=[nps], writes=[kvn_bc])

            P.barrier()
            xg = [P.sb(f"xg{i}", [128, 2, D], F32, s2) for i in range(2)]
            junk = P.sb("junk", [128, D], BF16, s2)
            ss = [P.sb(f"ss{i}", [128, 4], F32, s2) for i in range(2)]
            st2 = [P.sb(f"st2{i}", [128, 2], F32, s2) for i in range(2)]
            tmpf = [P.sb("tmpf0", [128, D], F32, s2)] * 2
            hb = [P.sb(f"hb{i}", [128, D], BF16, s2) for i in range(2)]
            hT = [P.sb("hT0", [128, 8, 512], BF16, s2)] * 2
            ckvb = [P.sb(f"ckvb{i}", [128, 256], BF16, s2) for i in range(2)]
            latk = [P.sb("latk0", [128, 256], F32, s2)] * 2
            latq = [P.sb("latq0", [128, 512], F32, s2)] * 2
            cqb = [P.sb(f"cqb{i}", [128, 512], BF16, s2) for i in range(2)]
            rope_t = [P.sb(f"rope_t{i}", [64, 2, 512], F32, s2) for i in range(2)]
            r1 = P.sb("r1", [64, 512], F32, s2)
            r2 = P.sb("r2", [64, 512], F32, s2)
            pT = [P.ps(f"pT{i}", [128, 1024], BF16, s2) for i in range(2)]
            pa = [P.ps(f"pa{i}", [128, 512], F32, s2) for i in range(2)]
            pb = [P.ps(f"pb{i}", [128, 512], F32, s2) for i in range(2)]
            pg = [P.ps(f"pg{i}", [128, 512], F32, s2) for i in range(2)]

            n_groups = 17
            tile_i = 0
            for g in range(n_groups):
                ntl = 4 if g < 16 else 2
                gw = ntl * 128
                own = g < 8 or g == 16
                gb = g % 2
                tok0 = g * 512
                is_ctx = g == 16
                A1 = bc[1][0] if is_ctx else bc[0][0]
                SH1 = bc[1][1] if is_ctx else bc[0][1]
                P.dma('pool', lambda e, gb=gb, tok0=tok0, gw=gw: e.dma_start(
                    out=rope_t[gb][:, :, 0:gw], in_=ropeT_k[:, :, tok0:tok0 + gw]), writes=[rope_t[gb]])
                for tl in range(ntl):
                    b2 = tile_i % 2
                    xb_ = xg[(tile_i // 2) % 2]
                    if tl % 2 == 0:
                        P.dma('sp', lambda e, xb_=xb_, tok0=tok0, gw=gw, ntl=ntl, tl=tl: e.dma_start(
                            out=xb_[:], in_=xk[tok0:tok0 + gw, :].rearrange("(p t) d -> p t d", t=ntl)[:, tl:tl + 2, :]), writes=[xb_])
                    xt_ = xb_[:, tl % 2, :]
                    P.op('act', lambda e, xt_=xt_, gb=gb, tl=tl: e.activation(out=junk[:], in_=xt_, func=AF.Square,
                                                                            accum_out=ss[gb][:, tl:tl + 1]),
                         reads=[xb_], writes=[junk, ss[gb]])
                    P.op('act', lambda e, gb=gb, tl=tl: e.activation(out=ss[gb][:, tl:tl + 1], in_=ss[gb][:, tl:tl + 1], func=AF.Sqrt,
                                                                   bias=epsc[:], scale=1.0 / D),
                         reads=[ss[gb], epsc], writes=[ss[gb]])
                    P.op('dve', lambda e, gb=gb, tl=tl: e.reciprocal(out=ss[gb][:, tl:tl + 1], in_=ss[gb][:, tl:tl + 1]),
                         reads=[ss[gb]], writes=[ss[gb]])
                    P.op('dve', lambda e, b2=b2, xt_=xt_, gb=gb, tl=tl, A1=A1: e.scalar_tensor_tensor(
                        out=tmpf[b2][:], in0=xt_, scalar=ss[gb][:, tl:tl + 1], in1=A1[:], op0=ALU.mult, op1=ALU.mult),
                        reads=[xb_, ss[gb], A1], writes=[tmpf[b2]])
                    P.op('pool', lambda e, b2=b2, SH1=SH1: e.tensor_tensor(out=hb[b2][:], in0=tmpf[b2][:], in1=SH1[:], op=ALU.add),
                         reads=[tmpf[b2], SH1], writes=[hb[b2]])
                    hbv = hb[b2][:].rearrange("t (k p) -> t k p", k=8)
                    for k in range(8):
                        P.op('pe', lambda e, b2=b2, k=k, hbv=hbv: e.transpose(out=pT[b2][:, k * 128:(k + 1) * 128],
                                                                            in_=hbv[:, k, :], identity=identb[:]),
                             reads=[hb[b2], identb], writes=[pT[b2]])
                    P.op('act', lambda e, b2=b2, gb=gb, tl=tl: e.activation(
                        out=hT[gb][:, :, tl * 128:(tl + 1) * 128], in_=pT[b2][:].rearrange("p (k t) -> p k t", k=8), func=AF.Copy),
                        reads=[pT[b2]], writes=[hT[gb]])
                    for k in range(8):
                        P.op('pe', lambda e, b2=b2, gb=gb, tl=tl, k=k: e.matmul(
                            pb[b2][:, 0:256], lhsT=hT[gb][:, k, tl * 128:(tl + 1) * 128], rhs=w_in_b[:, k, 512:768],
                            start=(k == 0), stop=(k == 7)), reads=[hT[gb], w_in_b], writes=[pb[b2]])
                    if own:
                        for k in range(8):
                            P.op('pe', lambda e, b2=b2, gb=gb, tl=tl, k=k: e.matmul(
                                pa[b2][:], lhsT=hT[gb][:, k, tl * 128:(tl + 1) * 128], rhs=w_in_b[:, k, 0:512],
                                start=(k == 0), stop=(k == 7)), reads=[hT[gb], w_in_b], writes=[pa[b2]])
                    s2_ = st2[b2]
                    P.op('act', lambda e, b2=b2: e.activation(out=latk[b2][:], in_=pb[b2][:, 0:256], func=AF.Copy),
                         reads=[pb[b2]], writes=[latk[b2]])
                    P.op('act', lambda e, b2=b2, s2_=s2_: e.activation(out=junk[:, 0:256], in_=latk[b2][:], func=AF.Square,
                                                                     accum_out=s2_[:, 0:1]),
                         reads=[latk[b2]], writes=[junk, s2_])
                    P.op('act', lambda e, s2_=s2_: e.activation(out=s2_[:, 0:1], in_=s2_[:, 0:1], func=AF.Sqrt, bias=epsc[:], scale=1.0 / 256),
                         reads=[s2_, epsc], writes=[s2_])
                    P.op('dve', lambda e, s2_=s2_: e.reciprocal(out=s2_[:, 0:1], in_=s2_[:, 0:1]), reads=[s2_], writes=[s2_])
                    P.op('dve', lambda e, b2=b2, s2_=s2_: e.scalar_tensor_tensor(
                        out=ckvb[b2][:], in0=latk[b2][:], scalar=s2_[:, 0:1], in1=kvn_bc[:], op0=ALU.mult, op1=ALU.mult),
                        reads=[latk[b2], s2_, kvn_bc], writes=[ckvb[b2]])
                    cv_ = ckvb[b2][:].rearrange("t (k p) -> t k p", k=2)
                    for k in range(2):
                        P.op('pe', lambda e, b2=b2, k=k, cv_=cv_: e.transpose(out=pT[b2][:, k * 128:(k + 1) * 128],
                                                                            in_=cv_[:, k, :], identity=identb[:]),
                             reads=[ckvb[b2], identb], writes=[pT[b2]])
                    c0 = tok0 + tl * 128
                    qcol = c0 if g < 8 else NOWN + tl * 128
                    P.op('act', lambda e, b2=b2, c0=c0: e.activation(
                        out=ckvT[:, :, c0:c0 + 128], in_=pT[b2][:, 0:256].rearrange("p (k t) -> p k t", k=2), func=AF.Copy),
                        reads=[pT[b2]], writes=[ckvT])
                    if own:
                        P.op('act', lambda e, b2=b2: e.activation(out=latq[b2][:], in_=pa[b2][:], func=AF.Copy),
                             reads=[pa[b2]], writes=[latq[b2]])
                        P.op('act', lambda e, b2=b2, s2_=s2_: e.activation(out=junk[:, 0:512], in_=latq[b2][:], func=AF.Square,
                                                                         accum_out=s2_[:, 1:2]),
                             reads=[latq[b2]], writes=[junk, s2_])
                        P.op('act', lambda e, s2_=s2_: e.activation(out=s2_[:, 1:2], in_=s2_[:, 1:2], func=AF.Sqrt, bias=epsc[:], scale=1.0 / 512),
                             reads=[s2_, epsc], writes=[s2_])
                        P.op('dve', lambda e, s2_=s2_: e.reciprocal(out=s2_[:, 1:2], in_=s2_[:, 1:2]), reads=[s2_], writes=[s2_])
                        P.op('dve', lambda e, b2=b2, s2_=s2_: e.scalar_tensor_tensor(
                            out=cqb[b2][:], in0=latq[b2][:], scalar=s2_[:, 1:2], in1=qn_bc[:], op0=ALU.mult, op1=ALU.mult),
                            reads=[latq[b2], s2_, qn_bc], writes=[cqb[b2]])
                        cq_ = cqb[b2][:].rearrange("t (k p) -> t k p", k=4)
                        for k in range(4):
                            P.op('pe', lambda e, b2=b2, k=k, cq_=cq_: e.transpose(out=pT[b2][:, k * 128:(k + 1) * 128],
                                                                                in_=cq_[:, k, :], identity=identb[:]),
                                 reads=[cqb[b2], identb], writes=[pT[b2]])
                        P.op('act', lambda e, b2=b2, qcol=qcol: e.activation(
                            out=cqT[:, :, qcol:qcol + 128], in_=pT[b2][:, 0:512].rearrange("p (k t) -> p k t", k=4), func=AF.Copy),
                            reads=[pT[b2]], writes=[cqT])
                    tile_i += 1
                pk, pk2 = pg[0], pg[1]
                for k in range(8):
                    P.op('pe', lambda e, gb=gb, k=k, gw=gw: e.matmul(pk[0:64, 0:gw], lhsT=w_in_b[:, k, 768:832], rhs=hT[gb][:, k, 0:gw],
                                                                    start=(k == 0), stop=(k == 7)),
                         reads=[w_in_b, hT[gb]], writes=[pk])
                for k in range(8):
                    P.op('pe', lambda e, gb=gb, k=k, gw=gw: e.matmul(pk2[0:64, 0:gw], lhsT=w_sw_b[:, k, :], rhs=hT[gb][:, k, 0:gw],
                                                                    start=(k == 0), stop=(k == 7)),
                         reads=[w_sw_b, hT[gb]], writes=[pk2])
                P.op('dve', lambda e, gb=gb, gw=gw: e.tensor_tensor(out=r1[:, 0:gw], in0=pk[0:64, 0:gw], in1=rope_t[gb][:, 0, 0:gw], op=ALU.mult),
                     reads=[pk, rope_t[gb]], writes=[r1])
                P.op('dve', lambda e, gb=gb, gw=gw: e.tensor_tensor(out=r2[:, 0:gw], in0=pk2[0:64, 0:gw], in1=rope_t[gb][:, 1, 0:gw], op=ALU.mult),
                     reads=[pk2, rope_t[gb]], writes=[r2])
                P.op('pool', lambda e, gw=gw, tok0=tok0: e.tensor_tensor(out=krT[:, tok0:tok0 + gw], in0=r1[:, 0:gw], in1=r2[:, 0:gw], op=ALU.add),
                     reads=[r1, r2], writes=[krT])

        P.barrier()
        if stage_limit <= 2:
            ov = out.bitcast(BF16) if hasattr(out, 'bitcast') else None
            P.dma('sp', lambda e: e.dma_start(out=dbg_o[0][0:128, :], in_=ckvT[:, 0, 0:4096]), reads=[ckvT])
            P.dma('sp', lambda e: e.dma_start(out=dbg_o[0][128:256, :], in_=ckvT[:, 1, 0:4096]), reads=[ckvT])
            P.dma('sp', lambda e: e.dma_start(out=dbg_o[0][256:320, :], in_=krT[:, 0:4096]), reads=[krT])
            for k in range(4):
                P.dma('sp', lambda e, k=k: e.dma_start(out=dbg_o[0][384 + k * 128:384 + (k + 1) * 128, :], in_=cqT[:, k, 0:4096]), reads=[cqT])
            P.dma('sp', lambda e: e.dma_start(out=dbg_o[0][1024:1152, :], in_=ckvT[:, 0, 4352:8448]), reads=[ckvT])
            P.finish()
            P.emit()
            print("instr counts", P.cnt, "waits", P.n_wait, "dmas", P.n_dma)
            return nc


        OT_d = P.dram("OT_d", [9, 128, H, 512], BF16)
        with ExitStack() as s3:
            KTn_h = P.sb("KTn_h", [128, NKEY], BF16, s3)
            Vh = P.sb("Vh", [128, NT_KEY, 128], BF16, s3)
            ropeq = [P.sb(f"ropeq{i}", [64, 2, 512], F32, s3) for i in range(2)]
            qn = [P.sb(f"qn{i}", [128, 512], BF16, s3) for i in range(2)]
            qr = [P.sb(f"qr{i}", [64, 512], BF16, s3) for i in range(2)]
            q1 = P.sb("q1", [64, 512], F32, s3)
            q2 = P.sb("q2", [64, 512], F32, s3)
            PT = [P.sb(f"PT{i}", [128, 1024], BF16, s3) for i in range(2)]
            acc = P.sb("acc", [128, 1024], F32, s3)
            rinv = P.sb("rinv", [128, 512], F32, s3)
            OTst = [P.sb(f"OTst{i}", [128, 512], BF16, s3) for i in range(2)]
            sps = [P.ps(f"sps{i}", [128, 1024], F32, s3) for i in range(2)]
            ops = [P.ps(f"ops{i}", [128, 512], F32, s3) for i in range(2)]
            pjs = [P.ps(f"pjs{i}", [128, 512], F32, s3) for i in range(2)]
            pji = 0
            evi = 0

            def evac(dst_ap, src_ap, rd, wr):
                nonlocal evi
                evi += 1
                if evi % 2 == 0:
                    P.op('act', lambda e: e.activation(out=dst_ap, in_=src_ap, func=AF.Copy), reads=rd, writes=wr)
                else:
                    P.op('dve', lambda e: e.tensor_copy(out=dst_ap, in_=src_ap), reads=rd, writes=wr)

            it = 0
            for h in range(H):
                for g in range(17):
                    gw = 512 if g < 16 else 256
                    c0 = g * 512
                    pj = pjs[pji % 2]; pji += 1
                    for k in range(2):
                        P.op('pe', lambda e, pj=pj, k=k, h=h, c0=c0, gw=gw: e.matmul(
                            pj[:, 0:gw], lhsT=w_uk_b[:, k, h * 128:(h + 1) * 128], rhs=ckvT[:, k, c0:c0 + gw], start=(k == 0), stop=(k == 1)),
                            reads=[w_uk_b, ckvT], writes=[pj])
                    evac(KTn_h[:, c0:c0 + gw], pj[:, 0:gw], [pj], [KTn_h])
                for t0 in range(0, NT_KEY, 4):
                    nt = min(4, NT_KEY - t0)
                    pj = pjs[pji % 2]; pji += 1
                    for j in range(nt):
                        t = t0 + j
                        for k in range(2):
                            P.op('pe', lambda e, pj=pj, j=j, t=t, k=k, h=h: e.matmul(
                                pj[:, j * 128:(j + 1) * 128], lhsT=ckvT[:, k, t * 128:(t + 1) * 128], rhs=w_uv_b[:, k, h * 128:(h + 1) * 128],
                                start=(k == 0), stop=(k == 1)), reads=[ckvT, w_uv_b], writes=[pj])
                    evac(Vh[:, t0:t0 + nt, :].rearrange("p t v -> p (t v)"), pj[:, 0:nt * 128], [pj], [Vh])
                for qb in range(9):
                    ib = it % 2
                    it += 1
                    qc0 = qb * 512
                    gps = list(range(33)) if qb < 8 else [32]
                    if qb < 8:
                        P.dma('pool', lambda e, ib=ib, qc0=qc0: e.dma_start(out=ropeq[ib][:], in_=ropeT_k[:, :, qc0:qc0 + 512]), writes=[ropeq[ib]])
                    else:
                        P.dma('pool', lambda e, ib=ib: e.dma_start(out=ropeq[ib][:, :, 0:LC], in_=ropeT_k[:, :, N:N + LC]), writes=[ropeq[ib]])
                    pj = pjs[pji % 2]; pji += 1
                    for k in range(4):
                        P.op('pe', lambda e, pj=pj, k=k, h=h, qc0=qc0: e.matmul(
                            pj[:], lhsT=w_uqn_b[:, k, h * 128:(h + 1) * 128], rhs=cqT[:, k, qc0:qc0 + 512], start=(k == 0), stop=(k == 3)),
                            reads=[w_uqn_b, cqT], writes=[pj])
                    evac(qn[ib][:], pj[:], [pj], [qn[ib]])
                    pjA = pjs[pji % 2]; pji += 1
                    pjB = pjs[pji % 2]; pji += 1
                    for k in range(4):
                        P.op('pe', lambda e, pjA=pjA, k=k, h=h, qc0=qc0: e.matmul(
                            pjA[0:64, :], lhsT=w_uqr_b[:, k, h * 64:(h + 1) * 64], rhs=cqT[:, k, qc0:qc0 + 512], start=(k == 0), stop=(k == 3)),
                            reads=[w_uqr_b, cqT], writes=[pjA])
                    for k in range(4):
                        P.op('pe', lambda e, pjB=pjB, k=k, h=h, qc0=qc0: e.matmul(
                            pjB[0:64, :], lhsT=w_uqs_b[:, k, h * 64:(h + 1) * 64], rhs=cqT[:, k, qc0:qc0 + 512], start=(k == 0), stop=(k == 3)),
                            reads=[w_uqs_b, cqT], writes=[pjB])
                    P.op('dve', lambda e, pjA=pjA, ib=ib: e.tensor_tensor(out=q1[:], in0=pjA[0:64, :], in1=ropeq[ib][:, 0, :], op=ALU.mult),
                         reads=[pjA, ropeq[ib]], writes=[q1])
                    P.op('dve', lambda e, pjB=pjB, ib=ib: e.tensor_tensor(out=q2[:], in0=pjB[0:64, :], in1=ropeq[ib][:, 1, :], op=ALU.mult),
                         reads=[pjB, ropeq[ib]], writes=[q2])
                    P.op('pool', lambda e, ib=ib: e.tensor_tensor(out=qr[ib][:], in0=q1[:], in1=q2[:], op=ALU.add),
                         reads=[q1, q2], writes=[qr[ib]])
                    op_ = ops[ib]
                    for gp in gps:
                        sp_ = sps[gp % 2]
                        pt_ = PT[gp % 2]
                        for j in range(2):
                            t = 2 * gp + j
                            P.op('pe', lambda e, sp_=sp_, j=j, t=t, ib=ib: e.matmul(
                                sp_[:, j * 512:(j + 1) * 512], lhsT=KTn_h[:, t * 128:(t + 1) * 128], rhs=qn[ib][:], start=True, stop=False),
                                reads=[KTn_h, qn[ib]], writes=[sp_])
                            P.op('pe', lambda e, sp_=sp_, j=j, t=t, ib=ib: e.matmul(
                                sp_[:, j * 512:(j + 1) * 512], lhsT=krT[:, t * 128:(t + 1) * 128], rhs=qr[ib][:], start=False, stop=True),
                                reads=[krT, qr[ib]], writes=[sp_])
                        P.op('act', lambda e, sp_=sp_, pt_=pt_: e.activation(out=pt_[:], in_=sp_[:], func=AF.Exp, scale=SCALE),
                             reads=[sp_], writes=[pt_])
                        if gp == gps[0]:
                            P.op('dve', lambda e, pt_=pt_: e.tensor_copy(out=acc[:], in_=pt_[:]), reads=[pt_], writes=[acc])
                        else:
                            P.op('dve', lambda e, pt_=pt_: e.tensor_tensor(out=acc[:], in0=acc[:], in1=pt_[:], op=ALU.add),
                                 reads=[pt_, acc], writes=[acc])
                        for j in range(2):
                            t = 2 * gp + j
                            P.op('pe', lambda e, op_=op_, pt_=pt_, j=j, t=t, gp=gp, g0=gps[0]: e.matmul(
                                op_[:], lhsT=Vh[:, t, :], rhs=pt_[:, j * 512:(j + 1) * 512],
                                start=(gp == g0 and j == 0), stop=(gp == 32 and j == 1)),
                                reads=[Vh, pt_], writes=[op_])
                    pj = pjs[pji % 2]; pji += 1
                    for j in range(2):
                        P.op('pe', lambda e, pj=pj, j=j: e.matmul(pj[:], lhsT=ones_f[:], rhs=acc[:, j * 512:(j + 1) * 512],
                                                                 start=(j == 0), stop=(j == 1)),
                             reads=[ones_f, acc], writes=[pj])
                    P.op('dve', lambda e, pj=pj: e.reciprocal(out=rinv[:], in_=pj[:]), reads=[pj], writes=[rinv])
                    P.op('dve', lambda e, op_=op_, ib=ib: e.tensor_tensor(out=OTst[ib][:], in0=op_[:], in1=rinv[:], op=ALU.mult),
                         reads=[op_, rinv], writes=[OTst[ib]])
                    P.dma('sp', lambda e, ib=ib, qb=qb, h=h: e.dma_start(out=OT_d[qb, :, h, :], in_=OTst[ib][:]),
                          reads=[OTst[ib]], writes=[OT_d])
        P.barrier()
        att.close()

        with ExitStack() as s4:
            w_o_b = P.sb("w_o_b", [128, H, D], BF16, s4)
            with ExitStack() as s4w:
                wst = P.sb("wst", [128, H, D], F32, s4w)
                P.dma('sp', lambda e: e.dma_start(out=wst[:], in_=w_o.rearrange("(h p) n -> p h n", p=128)), writes=[wst])
                P.op('pool', lambda e: e.tensor_copy(out=w_o_b[:], in_=wst[:]), reads=[wst], writes=[w_o_b])
            P.barrier()
            OTg = [P.sb(f"OTg{i}", [128, H, 512], BF16, s4) for i in range(2)]
            xg4 = [P.sb(f"xg4{i}", [128, 4, D], F32, s4) for i in range(2)]
            x1g = [P.sb(f"x1g{i}", [128, 4, D], F32, s4) for i in range(2)]
            tmp4 = [P.sb(f"tmp4{i}", [128, 512], F32, s4) for i in range(2)]
            py = [P.ps(f"py{i}", [128, 512], F32, s4) for i in range(2)]
            ii = 0
            for qb in range(9):
                gb = qb % 2
                tok0 = qb * 512
                ntl4 = 4 if qb < 8 else 2
                g1_ = g1_bc if qb < 8 else g1c_bc
                P.dma('sp', lambda e, gb=gb, qb=qb: e.dma_start(out=OTg[gb][:], in_=OT_d[qb]), reads=[OT_d], writes=[OTg[gb]])
                if qb < 8:
                    P.dma('pool', lambda e, gb=gb, tok0=tok0: e.dma_start(
                        out=xg4[gb][:], in_=xk[tok0:tok0 + 512, :].rearrange("(p t) d -> p t d", t=4)), writes=[xg4[gb]])
                else:
                    P.dma('pool', lambda e, gb=gb: e.dma_start(
                        out=xg4[gb][:, 0:2, :], in_=xk[N:N + LC, :].rearrange("(p t) d -> p t d", t=2)), writes=[xg4[gb]])
                for tl in range(ntl4):
                    for cb in range(2):
                        p_ = py[ii % 2]
                        t_ = tmp4[ii % 2]
                        ii += 1
                        for h in range(H):
                            P.op('pe', lambda e, p_=p_, gb=gb, h=h, tl=tl, cb=cb: e.matmul(
                                p_[:], lhsT=OTg[gb][:, h, tl * 128:(tl + 1) * 128], rhs=w_o_b[:, h, cb * 512:(cb + 1) * 512],
                                start=(h == 0), stop=(h == H - 1)), reads=[OTg[gb], w_o_b], writes=[p_])
                        P.op('dve', lambda e, p_=p_, t_=t_, cb=cb, g1_=g1_: e.tensor_tensor(out=t_[:], in0=p_[:], in1=g1_[:, cb * 512:(cb + 1) * 512], op=ALU.mult),
                             reads=[p_, g1_], writes=[t_])
                        P.op('pool', lambda e, t_=t_, gb=gb, tl=tl, cb=cb: e.tensor_tensor(
                            out=x1g[gb][:, tl, cb * 512:(cb + 1) * 512], in0=xg4[gb][:, tl, cb * 512:(cb + 1) * 512], in1=t_[:], op=ALU.add),
                            reads=[t_, xg4[gb]], writes=[x1g[gb]])
                P.dma('sp', lambda e, gb=gb, tok0=tok0, ntl4=ntl4: e.dma_start(
                    out=X1_d[tok0:tok0 + ntl4 * 128, :].rearrange("(t p) d -> p t d", p=128), in_=x1g[gb][:, 0:ntl4, :]), reads=[x1g[gb]], writes=[X1_d])
        P.barrier()

        if stage_limit == 4:
            with ExitStack() as sf:
                cp = [P.sb(f"cp{i}", [128, D], F32, sf) for i in range(2)]
                for t in range(NT_OWN):
                    P.dma('sp', lambda e, t=t: e.dma_start(out=cp[t % 2][:], in_=X1_d[t * 128:(t + 1) * 128, :]), reads=[X1_d], writes=[cp[t % 2]])
                    P.dma('pool', lambda e, t=t: e.dma_start(out=out[t * 128:(t + 1) * 128, :], in_=cp[t % 2][:]), reads=[cp[t % 2]])
            P.finish()
            P.emit()
            return nc
        m2 = [[P.sb(f"m2_{j}_{v}", [128, D], F32) for v in range(3)] for j in range(2)]
        emit_mod(P, nc, cvec, mod_w[0], mod_b[0], norm1[0], norm2[0], ones_f,
                 [(j, 3 + v, m2[j][v]) for j in range(2) for v in range(3)])

        def writer0(P, t, xo):
            if t < NT_OWN:
                qb, tl = t // 4, t % 4
                dst = out[qb * 512:(qb + 1) * 512, :].rearrange("(p t) d -> p t d", t=4)[:, tl, :]
            else:
                dst = outc.rearrange("(p t) d -> p t d", t=2)[:, t - NT_OWN, :]
            P.dma('pool', lambda e: e.dma_start(out=dst, in_=xo[:]), reads=[xo])

        emit_moe(P, nc, X1_d.t, NT_OWN + 2, lambda t: tuple(m2[0 if t < NT_OWN else 1]), m_wr, m_br, m_wgu, m_bgu, m_wd, m_bd,
                 ident, identb, ones_f, epsc, writer0)

        P.finish()
        P.emit()
        print("instr counts", P.cnt, "waits", P.n_wait, "dmas", P.n_dma)
    return nc


def _rope_tables_T():
    rows = N // 64
    row = np.repeat(np.arange(rows, dtype=np.float32), 64)
    col = np.tile(np.arange(64, dtype=np.float32), rows)
    nf = 16
    inv = np.power(np.float32(10000.0), -np.arange(nf, dtype=np.float32) / nf).astype(np.float32)
    ang_r = row[:, None] * inv
    ang_c = col[:, None] * inv
    cos = np.concatenate([np.cos(ang_r), np.cos(ang_r), np.cos(ang_c), np.cos(ang_c)], axis=1)
    sin = np.concatenate([-np.sin(ang_r), np.sin(ang_r), -np.sin(ang_c), np.sin(ang_c)], axis=1)
    return np.stack([cos.T, sin.T]).astype(np.float32)


def _dev_order():
    idx = []
    for g in range(17):
        ntl = 4 if g < 16 else 2
        tok0 = g * 512
        for tl in range(ntl):
            idx.append(tok0 + np.arange(128) * ntl + tl)
    return np.concatenate(idx)


_DEV_ORDER = _dev_order()
_SWAP64 = np.concatenate([np.arange(16, 32), np.arange(0, 16), np.arange(48, 64), np.arange(32, 48)])


def make_in_maps(inp):
    f = lambda a: np.ascontiguousarray(np.asarray(a, dtype=np.float32))
    x = f(inp['x']); ctx = f(inp['ctx']); c = f(inp['c']); c_ctx = f(inp['c_ctx'])
    rt = _rope_tables_T()
    ident = np.eye(128, dtype=np.float32)
    w_in = f(inp['mla_w_in'][0])
    w_uq = f(inp['mla_w_uq'][0]).reshape(512, H, 192)
    w_ukv = f(inp['mla_w_ukv'][0])
    shared = {
        "ident": ident,
        "mod_w": f(inp['mod_w']), "mod_b": f(inp['mod_b']).reshape(2, 1, 6 * D),
        "norm1": f(inp['norm1']).reshape(2, 1, D), "norm2": f(inp['norm2']).reshape(2, 1, D),
        "mla_w_in": w_in, "mla_w_kr_sw": np.ascontiguousarray(w_in[:, 768:832][:, _SWAP64]),
        "mla_q_norm": f(inp['mla_q_norm']).reshape(1, 512), "mla_kv_norm": f(inp['mla_kv_norm']).reshape(1, 256),
        "w_uq_nope": np.ascontiguousarray(w_uq[:, :, :128].reshape(512, H * 128)),
        "w_uq_rope": np.ascontiguousarray(w_uq[:, :, 128:].reshape(512, H * 64)),
        "w_uq_rope_sw": np.ascontiguousarray(w_uq[:, :, 128:][:, :, _SWAP64].reshape(512, H * 64)),
        "w_uk": np.ascontiguousarray(w_ukv[:, :H * 128]), "w_uv": np.ascontiguousarray(w_ukv[:, H * 128:]),
        "mla_w_o": f(inp['mla_w_o'][0]),
        "moe_w_router": f(inp['moe_w_router'][0]), "moe_b_router": f(inp['moe_b_router'][0]).reshape(1, NE),
        "moe_w_gu": f(inp['moe_w_gu'][0]), "moe_b_gu": f(inp['moe_b_gu'][0]),
        "moe_w_down": f(inp['moe_w_down'][0]), "moe_b_down": f(inp['moe_b_down'][0]),
    }
    maps = []
    for core in range(8):
        b, half = core // 2, core % 2
        own = slice(half * NOWN, (half + 1) * NOWN)
        oth = slice((1 - half) * NOWN, (2 - half) * NOWN)
        xk = np.concatenate([x[b, own], x[b, oth], ctx[b]], axis=0)
        ropeT = np.concatenate([rt[:, :, own], rt[:, :, oth],
                                np.stack([np.ones((64, LC), np.float32), np.zeros((64, LC), np.float32)])], axis=2)
        ropeT = np.ascontiguousarray(ropeT[:, :, _DEV_ORDER].transpose(1, 0, 2))
        cvec = np.stack([c[b].reshape(8, 128).T, c_ctx.reshape(8, 128).T], axis=2)
        m = dict(shared)
        m.update({"xk": np.ascontiguousarray(xk), "ropeT_k": np.ascontiguousarray(ropeT), "cvec": np.ascontiguousarray(cvec)})
        maps.append(m)
    return maps


GH = 4
GDK = 128
GDV = 256


def emit_mod(P, nc, cvec, mod_w_l, mod_b_l, n1_l, n2_l, ones_f, want, tag="m"):
    with ExitStack() as s1:
        cv = P.sb(tag + "cv", [128, 8, 2], F32, s1)
        scv = P.sb(tag + "scv", [128, 8, 2], F32, s1)
        modrow = [P.sb(tag + f"modrow{j}", [1, 6 * D], F32, s1) for j in range(2)]
        mb = P.sb(tag + "mb", [1, 6 * D], F32, s1)
        n1 = P.sb(tag + "n1", [1, D], F32, s1)
        n2 = P.sb(tag + "n2", [1, D], F32, s1)
        wblk = [P.sb(tag + f"wblk{i}", [128, 8, 512], F32, s1) for i in range(2)]
        mps = [P.ps(tag + f"mps{j}", [1, 512], F32, s1) for j in range(2)]
        bps = [P.ps(tag + f"bps{i}", [128, 512], F32, s1) for i in range(2)]
        P.dma('sp', lambda e: e.dma_start(out=cv[:], in_=cvec[:, :, :]), writes=[cv])
        P.dma('sp', lambda e: e.dma_start(out=mb[:], in_=mod_b_l), writes=[mb])
        P.dma('sp', lambda e: e.dma_start(out=n1[:], in_=n1_l), writes=[n1])
        P.dma('sp', lambda e: e.dma_start(out=n2[:], in_=n2_l), writes=[n2])
        P.op('act', lambda e: e.activation(out=scv[:], in_=cv[:], func=AF.Silu), reads=[cv], writes=[scv])
        mw = mod_w_l.rearrange("(k p) n -> p k n", p=128)
        for cb in range(12):
            wb_ = wblk[cb % 2]
            P.dma('sp' if cb % 2 == 0 else 'pool',
                  lambda e, wb_=wb_, cb=cb: e.dma_start(out=wb_[:], in_=mw[:, :, cb * 512:(cb + 1) * 512]), writes=[wb_])
            for j in range(2):
                for k in range(8):
                    P.op('pe', lambda e, j=j, k=k, wb_=wb_: e.matmul(mps[j][:], lhsT=scv[:, k, j:j + 1], rhs=wb_[:, k, :],
                                                                    start=(k == 0), stop=(k == 7)),
                         reads=[scv, wb_], writes=[mps[j]])
                P.op('dve', lambda e, j=j, cb=cb: e.tensor_tensor(out=modrow[j][:, cb * 512:(cb + 1) * 512], in0=mps[j][:],
                                                                 in1=mb[:, cb * 512:(cb + 1) * 512], op=ALU.add),
                     reads=[mps[j], mb], writes=[modrow[j]])
        for j in range(2):
            for (lo, nrm) in ((D, n1), (4 * D, n2)):
                P.op('dve', lambda e, j=j, lo=lo, nrm=nrm: e.scalar_tensor_tensor(
                    out=modrow[j][:, lo:lo + D], in0=modrow[j][:, lo:lo + D], scalar=1.0, in1=nrm[:],
                    op0=ALU.add, op1=ALU.mult), reads=[modrow[j], nrm], writes=[modrow[j]])
        seg = {0: 1, 1: 0, 2: 2, 3: 4, 4: 3, 5: 5}
        i = 0
        for (j, v, dst) in want:
            for hf in range(2):
                lo = seg[v] * D + hf * 512
                bp = bps[i % 2]
                P.op('pe', lambda e, bp=bp, j=j, lo=lo: e.matmul(bp[:], lhsT=ones_f[0:1, :], rhs=modrow[j][:, lo:lo + 512],
                                                               start=True, stop=True),
                     reads=[ones_f, modrow[j]], writes=[bp])
                P.op('act', lambda e, bp=bp, dst=dst, hf=hf: e.activation(out=dst[:, hf * 512:(hf + 1) * 512], in_=bp[:], func=AF.Copy),
                     reads=[bp], writes=[dst])
                i += 1
    P.barrier()


def build_layer1(stage_limit=99):
    nc = bass.Bass("TRN2", target_bir_lowering=False)

    def din(name, shape, dt=F32):
        return nc.dram_tensor(name, list(shape), dt, kind="ExternalInput").ap()

    NTOK = LC + N
    xs = din("xs", [NTOK, D])
    cvec = din("cvec", [128, 8, 2])
    ident_d = din("ident", [128, 128])
    tri_d = din("tri", [2, 128, 128])
    mod_w = din("mod_w1", [D, 6 * D])
    mod_b = din("mod_b1", [1, 6 * D])
    norm1 = din("norm1_1", [1, D])
    norm2 = din("norm2_1", [1, D])
    w_in = din("gla_w_in", [D, 3072])
    w_g1 = din("gla_w_g1", [2, D, 16])
    w_g2 = din("gla_w_g2", [2, 16, 512])
    b_g = din("gla_b_g", [2, 1, 512])
    hnorm = din("gla_hn", [1, 1024])
    w_o = din("gla_w_o", [D, D])
    fnorm = din("final_norm", [1, D])
    m_wr = din("moe_w_router", [D, NE])
    m_br = din("moe_b_router", [1, NE])
    m_wgu = din("moe_w_gu", [NE, D, 2 * D])
    m_bgu = din("moe_b_gu", [NE, 2 * D])
    m_wd = din("moe_w_down", [NE, D, D])
    m_bd = din("moe_b_down", [NE, D])
    out = nc.dram_tensor("out", [NOWN, D], F32, kind="ExternalOutput").ap()

    with ExitStack() as st:
        P = Prog(nc, st)
        oA_d = P.dram("oA_d", [32, 128, 1024], F32)
        ident = P.sb("ident", [128, 128], F32)
        identb = P.sb("identb", [128, 128], BF16)
        ones_f = P.sb("ones_f", [128, 128], F32)
        ones_b = P.sb("ones_b", [1, 128], BF16)
        epsc = P.sb("epsc", [128, 1], F32)
        trif = [P.sb(f"trif{d}", [128, 128], F32) for d in range(2)]
        negc = P.sb("negc", [128, 128], F32)
        mask4 = [P.sb(f"mask4{d}", [128, 4, 128], F32) for d in range(2)]
        P.dma('sp', lambda e: e.dma_start(out=ident[:], in_=ident_d[:, :]), writes=[ident])
        P.op('dve', lambda e: e.tensor_copy(out=identb[:], in_=ident[:]), reads=[ident], writes=[identb])
        P.op('pool', lambda e: e.memset(ones_f[:], 1.0), writes=[ones_f])
        P.op('pool', lambda e: e.memset(ones_b[:], 1.0), writes=[ones_b])
        P.op('pool', lambda e: e.memset(epsc[:], EPS), writes=[epsc])
        P.op('pool', lambda e: e.memset(negc[:], -1.0 / 16.0), writes=[negc])
        for d in range(2):
            for hh in range(4):
                P.dma('sp', lambda e, d=d, hh=hh: e.dma_start(out=mask4[d][:, hh, :], in_=tri_d[d]), writes=[mask4[d]])
            P.op('dve', lambda e, d=d: e.tensor_scalar(out=trif[d][:], in0=mask4[d][:, 0, :], scalar1=-1.0 / 16.0, scalar2=None, op0=ALU.mult),
                 reads=[mask4[d]], writes=[trif[d]])

        A2m = P.sb("A2m", [128, D], F32)
        SH2m = P.sb("SH2m", [128, D], F32)
        G2m = P.sb("G2m", [128, D], F32)
        fn_bc = P.sb("fn_bc", [128, D], F32)
        X2_d = P.dram("X2_d", [NOWN, D], F32)
        sgl = ExitStack()
        st.enter_context(sgl)
        base_stack = P.stack
        P.stack = sgl
        A1 = [P.sb(f"A1_{j}", [128, D], F32) for j in range(2)]
        SH1 = [P.sb(f"SH1_{j}", [128, D], F32) for j in range(2)]
        G1 = P.sb("G1", [128, D], F32)
        emit_mod(P, nc, cvec, mod_w, mod_b, norm1, norm2, ones_f,
                 [(0, 0, A1[0]), (0, 1, SH1[0]), (1, 0, A1[1]), (1, 1, SH1[1]), (0, 2, G1),
                  (0, 3, A2m), (0, 4, SH2m), (0, 5, G2m)])

        w_in_b = P.sb("w_in_b", [128, 8, 3072], BF16)
        w_g1_b = P.sb("w_g1_b", [128, 2, 8, 16], BF16)
        w_g2_b = P.sb("w_g2_b", [16, 2, 512], BF16)
        bg_b = P.sb("bg_b", [1, 2, 512], BF16)
        w_o_b = P.sb("w_o_b", [128, 8, D], BF16)
        hn_bc = P.sb("hn_bc", [128, 1024], F32)
        with ExitStack() as sw:
            stg = P.sb("stg", [128, 8, 1024], F32, sw)
            for cbk in range(3):
                P.dma('sp', lambda e, cbk=cbk: e.dma_start(out=stg[:], in_=w_in.rearrange("(k p) n -> p k n", p=128)[:, :, cbk * 1024:(cbk + 1) * 1024]),
                      writes=[stg])
                P.op('pool', lambda e, cbk=cbk: e.tensor_copy(out=w_in_b[:, :, cbk * 1024:(cbk + 1) * 1024], in_=stg[:]), reads=[stg], writes=[w_in_b])
            P.dma('sp', lambda e: e.dma_start(out=stg[:], in_=w_o.rearrange("(k p) n -> p k n", p=128)), writes=[stg])
            P.op('pool', lambda e: e.tensor_copy(out=w_o_b[:], in_=stg[:]), reads=[stg], writes=[w_o_b])
            g1s = P.sb("g1s", [128, 2, 8, 16], F32, sw)
            g2s = P.sb("g2s", [16, 2, 512], F32, sw)
            bgs = P.sb("bgs", [1, 2, 512], F32, sw)
            hns = P.sb("hns", [1, 1024], F32, sw)
            hps = P.ps("hps", [128, 512], F32, sw)
            P.dma('sp', lambda e: e.dma_start(out=g1s[:], in_=w_g1.rearrange("s (k p) r -> p s k r", p=128)), writes=[g1s])
            P.dma('sp', lambda e: e.dma_start(out=g2s[:], in_=w_g2.rearrange("s r n -> r s n")), writes=[g2s])
            P.dma('sp', lambda e: e.dma_start(out=bgs[:], in_=b_g.rearrange("s o n -> o s n")), writes=[bgs])
            P.dma('sp', lambda e: e.dma_start(out=hns[:], in_=hnorm[:, :]), writes=[hns])
            P.op('pool', lambda e: e.tensor_copy(out=w_g1_b[:], in_=g1s[:]), reads=[g1s], writes=[w_g1_b])
            P.op('pool', lambda e: e.tensor_copy(out=w_g2_b[:], in_=g2s[:]), reads=[g2s], writes=[w_g2_b])
            P.op('pool', lambda e: e.tensor_copy(out=bg_b[:], in_=bgs[:]), reads=[bgs], writes=[bg_b])
            for hf in range(2):
                P.op('pe', lambda e, hf=hf: e.matmul(hps[:], lhsT=ones_f[0:1, :], rhs=hns[:, hf * 512:(hf + 1) * 512], start=True, stop=True),
                     reads=[ones_f, hns], writes=[hps])
                P.op('act', lambda e, hf=hf: e.activation(out=hn_bc[:, hf * 512:(hf + 1) * 512], in_=hps[:], func=AF.Copy), reads=[hps], writes=[hn_bc])
        P.barrier()

        S = P.sb("S", [128, GH, GDV], F32)
        S_b = P.sb("S_b", [128, GH, GDV], BF16)
        xt = [P.sb(f"xt{i}", [128, D], F32) for i in range(2)]
        junk = P.sb("junk", [128, D], BF16)
        ss = P.sb("ss", [128, 8], F32)
        tmpf = P.sb("tmpf", [128, D], F32)
        hb = P.sb("hb", [128, D], BF16)
        hT = P.sb("hT", [128, 8, 128], BF16)
        z1T = P.sb("z1T", [16, 128], BF16)
        tex = P.sb("tex", [128, 512], F32)
        ln1p = P.sb("ln1p", [128, 512], F32)
        b_sb = P.sb("b_sb", [128, 512], F32)
        eb = P.sb("eb", [128, 512], F32)
        enb = P.sb("enb", [128, 512], F32)
        ek2 = P.sb("ek2", [128, 512], F32)
        qp = P.sb("qp", [128, 512], BF16)
        kp = P.sb("kp", [128, 512], BF16)
        kpp = P.sb("kpp", [128, 512], BF16)
        v_bf = P.sb("v_bf", [128, 1024], BF16)
        qkT = P.sb("qkT", [128, 8, 128], BF16)
        ATm = P.sb("ATm", [128, 4, 128], BF16)
        dcol = P.sb("dcol", [128, 4], F32)
        o_sb = P.sb("o_sb", [128, 1024], F32)
        oA = P.sb("oA", [128, 1024], F32)
        sr = P.sb("sr", [128, 1024], F32)
        gated = P.sb("gated", [128, 1024], BF16)
        gT = P.sb("gT", [128, 8, 128], BF16)
        x2 = [P.sb(f"x2{i}", [128, D], F32) for i in range(2)]
        pT = P.ps("pT", [128, 1024], BF16)
        B1 = P.ps("B1", [128, 512], F32)
        B2 = P.ps("B2", [128, 512], F32)
        P34 = P.ps("P34", [128, 1024], F32)
        B5 = P.ps("B5", [128, 512], F32)
        P67 = P.ps("P67", [128, 1024], F32)
        P.op('pool', lambda e: e.memset(S[:], 0.0), writes=[S])
        P.op('pool', lambda e: e.memset(S_b[:], 0.0), writes=[S_b])

        def chunk(tok0, is_ctx, d, want_out, final, own_idx, ci):
            xb = xt[ci % 2]
            a1, sh1 = (A1[1], SH1[1]) if is_ctx else (A1[0], SH1[0])
            P.dma('sp', lambda e: e.dma_start(out=xb[:], in_=xs[tok0:tok0 + 128, :]), writes=[xb])
            P.op('act', lambda e: e.activation(out=junk[:], in_=xb[:], func=AF.Square, accum_out=ss[:, 0:1]), reads=[xb], writes=[junk, ss])
            P.op('act', lambda e: e.activation(out=ss[:, 0:1], in_=ss[:, 0:1], func=AF.Sqrt, bias=epsc[:], scale=1.0 / D), reads=[ss, epsc], writes=[ss])
            P.op('dve', lambda e: e.reciprocal(out=ss[:, 0:1], in_=ss[:, 0:1]), reads=[ss], writes=[ss])
            P.op('dve', lambda e: e.scalar_tensor_tensor(out=tmpf[:], in0=xb[:], scalar=ss[:, 0:1], in1=a1[:], op0=ALU.mult, op1=ALU.mult),
                 reads=[xb, ss, a1], writes=[tmpf])
            P.op('pool', lambda e: e.tensor_tensor(out=hb[:], in0=tmpf[:], in1=sh1[:], op=ALU.add), reads=[tmpf, sh1], writes=[hb])
            for k in range(8):
                P.op('pe', lambda e, k=k: e.transpose(out=pT[:, k * 128:(k + 1) * 128], in_=hb[:, k * 128:(k + 1) * 128], identity=identb[:]),
                     reads=[hb, identb], writes=[pT])
            P.op('act', lambda e: e.activation(out=hT[:].rearrange("p k t -> p (k t)"), in_=pT[:], func=AF.Copy), reads=[pT], writes=[hT])
            for k in range(8):
                P.op('pe', lambda e, k=k: e.matmul(B1[0:16, 0:128], lhsT=w_g1_b[:, d, k, :], rhs=hT[:, k, :], start=(k == 0), stop=(k == 7)),
                     reads=[w_g1_b, hT], writes=[B1])
            P.op('act', lambda e: e.activation(out=z1T[:], in_=B1[0:16, 0:128], func=AF.Copy), reads=[B1], writes=[z1T])
            P.op('pe', lambda e: e.matmul(B5[:], lhsT=z1T[:], rhs=w_g2_b[:, d, :], start=True, stop=False), reads=[z1T, w_g2_b], writes=[B5])
            P.op('pe', lambda e: e.matmul(B5[:], lhsT=ones_b[:], rhs=bg_b[:, d, :], start=False, stop=True), reads=[ones_b, bg_b], writes=[B5])
            P.op('act', lambda e: e.activation(out=tex[:], in_=B5[:], func=AF.Exp, scale=-1.0), reads=[B5], writes=[tex])
            P.op('act', lambda e: e.activation(out=ln1p[:], in_=tex[:], func=AF.Ln, bias=1.0), reads=[tex], writes=[ln1p])
            P.op('pe', lambda e: e.matmul(B5[:], lhsT=trif[d][:], rhs=ln1p[:], start=True, stop=True), reads=[trif[d], ln1p], writes=[B5])
            P.op('pe', lambda e: e.matmul(B2[:], lhsT=negc[:], rhs=ln1p[:], start=True, stop=True), reads=[negc, ln1p], writes=[B2])
            P.op('act', lambda e: e.activation(out=b_sb[:], in_=B5[:], func=AF.Copy), reads=[B5], writes=[b_sb])
            P.op('act', lambda e: e.activation(out=enb[:], in_=b_sb[:], func=AF.Exp, scale=-1.0), reads=[b_sb], writes=[enb])
            P.op('dve', lambda e: e.tensor_tensor(out=ek2[:], in0=B2[:], in1=b_sb[:], op=ALU.subtract), reads=[B2, b_sb], writes=[ek2])
            P.op('act', lambda e: e.activation(out=ek2[:], in_=ek2[:], func=AF.Exp), reads=[ek2], writes=[ek2])
            for hh in range(GH):
                P.op('pe', lambda e, hh=hh: e.matmul(B1[:, 256 + hh:257 + hh], lhsT=ln1p[:, hh * 128:(hh + 1) * 128], rhs=negc[:, 0:1], start=True, stop=True),
                     reads=[ln1p, negc], writes=[B1])
            P.op('act', lambda e: e.activation(out=dcol[:], in_=B1[:, 256:260], func=AF.Exp), reads=[B1], writes=[dcol])
            for k in range(8):
                P.op('pe', lambda e, k=k: e.matmul(B1[:], lhsT=hT[:, k, :], rhs=w_in_b[:, k, 512:1024], start=(k == 0), stop=(k == 7)),
                     reads=[hT, w_in_b], writes=[B1])
            for cb in range(2):
                for k in range(8):
                    P.op('pe', lambda e, k=k, cb=cb: e.matmul(P34[:, cb * 512:(cb + 1) * 512], lhsT=hT[:, k, :],
                                                             rhs=w_in_b[:, k, 1024 + cb * 512:1536 + cb * 512], start=(k == 0), stop=(k == 7)),
                         reads=[hT, w_in_b], writes=[P34])
            P.op('act', lambda e: e.activation(out=v_bf[:], in_=P34[:], func=AF.Copy), reads=[P34], writes=[v_bf])
            P.op('dve', lambda e: e.tensor_tensor(out=kpp[:], in0=B1[:], in1=ek2[:], op=ALU.mult), reads=[B1, ek2], writes=[kpp])
            if want_out:
                P.op('act', lambda e: e.activation(out=eb[:], in_=b_sb[:], func=AF.Exp), reads=[b_sb], writes=[eb])
                P.op('dve', lambda e: e.tensor_tensor(out=kp[:], in0=B1[:], in1=enb[:], op=ALU.mult), reads=[B1, enb], writes=[kp])
                for k in range(8):
                    P.op('pe', lambda e, k=k: e.matmul(B2[:], lhsT=hT[:, k, :], rhs=w_in_b[:, k, 0:512], start=(k == 0), stop=(k == 7)),
                         reads=[hT, w_in_b], writes=[B2])
                P.op('dve', lambda e: e.scalar_tensor_tensor(out=qp[:], in0=B2[:], scalar=float(GDK ** -0.5), in1=eb[:], op0=ALU.mult, op1=ALU.mult),
                     reads=[B2, eb], writes=[qp])
                for hh in range(GH):
                    P.op('pe', lambda e, hh=hh: e.transpose(out=pT[:, hh * 128:(hh + 1) * 128], in_=qp[:, hh * 128:(hh + 1) * 128], identity=identb[:]),
                         reads=[qp, identb], writes=[pT])
                    P.op('pe', lambda e, hh=hh: e.transpose(out=pT[:, (4 + hh) * 128:(5 + hh) * 128], in_=kp[:, hh * 128:(hh + 1) * 128], identity=identb[:]),
                         reads=[kp, identb], writes=[pT])
                P.op('act', lambda e: e.activation(out=qkT[:].rearrange("p k t -> p (k t)"), in_=pT[:], func=AF.Copy), reads=[pT], writes=[qkT])
                for hh in range(GH):
                    P.op('pe', lambda e, hh=hh: e.matmul(B1[:, hh * 128:(hh + 1) * 128], lhsT=qkT[:, 4 + hh, :], rhs=qkT[:, hh, :], start=True, stop=True),
                         reads=[qkT], writes=[B1])
                P.op('dve', lambda e: e.tensor_tensor(out=ATm[:].rearrange("p h t -> p (h t)"), in0=B1[:], in1=mask4[d][:].rearrange("p h t -> p (h t)"), op=ALU.mult),
                     reads=[B1, mask4[d]], writes=[ATm])
                for hh in range(GH):
                    P.op('pe', lambda e, hh=hh: e.matmul(P67[:, hh * 256:(hh + 1) * 256], lhsT=ATm[:, hh, :], rhs=v_bf[:, hh * 256:(hh + 1) * 256], start=True, stop=False),
                         reads=[ATm, v_bf], writes=[P67])
                    P.op('pe', lambda e, hh=hh: e.matmul(P67[:, hh * 256:(hh + 1) * 256], lhsT=qkT[:, hh, :], rhs=S_b[:, hh, :], start=False, stop=True),
                         reads=[qkT, S_b], writes=[P67])
                if not final:
                    P.op('act', lambda e: e.activation(out=o_sb[:], in_=P67[:], func=AF.Copy), reads=[P67], writes=[o_sb])
                    P.dma('pool', lambda e: e.dma_start(out=oA_d[own_idx], in_=o_sb[:]), reads=[o_sb], writes=[oA_d])
                else:
                    P.dma('pool', lambda e: e.dma_start(out=oA[:], in_=oA_d[own_idx]), reads=[oA_d], writes=[oA])
                    P.op('dve', lambda e: e.tensor_tensor(out=o_sb[:], in0=P67[:], in1=oA[:], op=ALU.add), reads=[P67, oA], writes=[o_sb])
            for hh in range(GH):
                P.op('pe', lambda e, hh=hh: e.matmul(P34[:, hh * 256:(hh + 1) * 256], lhsT=kpp[:, hh * 128:(hh + 1) * 128], rhs=v_bf[:, hh * 256:(hh + 1) * 256],
                                                     start=True, stop=True), reads=[kpp, v_bf], writes=[P34])
            for hh in range(GH):
                P.op('dve', lambda e, hh=hh: e.scalar_tensor_tensor(out=S[:, hh, :], in0=S[:, hh, :], scalar=dcol[:, hh:hh + 1], in1=P34[:, hh * 256:(hh + 1) * 256],
                                                                    op0=ALU.mult, op1=ALU.add), reads=[S, dcol, P34], writes=[S])
            P.op('act', lambda e: e.activation(out=S_b[:].rearrange("p h v -> p (h v)"), in_=S[:].rearrange("p h v -> p (h v)"), func=AF.Copy), reads=[S], writes=[S_b])
            if want_out and final:
                for cb in range(2):
                    for k in range(8):
                        P.op('pe', lambda e, k=k, cb=cb: e.matmul(P34[:, cb * 512:(cb + 1) * 512], lhsT=hT[:, k, :],
                                                                 rhs=w_in_b[:, k, 2048 + cb * 512:2560 + cb * 512], start=(k == 0), stop=(k == 7)),
                             reads=[hT, w_in_b], writes=[P34])
                P.op('act', lambda e: e.activation(out=sr[:], in_=P34[:], func=AF.Silu), reads=[P34], writes=[sr])
                for hh in range(GH):
                    P.op('act', lambda e, hh=hh: e.activation(out=junk[:, 0:256], in_=o_sb[:, hh * 256:(hh + 1) * 256], func=AF.Square, accum_out=ss[:, 4 + hh:5 + hh]),
                         reads=[o_sb], writes=[junk, ss])
                P.op('act', lambda e: e.activation(out=ss[:, 4:8], in_=ss[:, 4:8], func=AF.Sqrt, bias=epsc[:], scale=1.0 / GDV), reads=[ss, epsc], writes=[ss])
                P.op('dve', lambda e: e.reciprocal(out=ss[:, 4:8], in_=ss[:, 4:8]), reads=[ss], writes=[ss])
                for hh in range(GH):
                    P.op('dve', lambda e, hh=hh: e.scalar_tensor_tensor(out=o_sb[:, hh * 256:(hh + 1) * 256], in0=o_sb[:, hh * 256:(hh + 1) * 256],
                                                                        scalar=ss[:, 4 + hh:5 + hh], in1=hn_bc[:, hh * 256:(hh + 1) * 256], op0=ALU.mult, op1=ALU.mult),
                         reads=[o_sb, ss, hn_bc], writes=[o_sb])
                P.op('pool', lambda e: e.tensor_tensor(out=gated[:], in0=o_sb[:], in1=sr[:], op=ALU.mult), reads=[o_sb, sr], writes=[gated])
                for k in range(8):
                    P.op('pe', lambda e, k=k: e.transpose(out=pT[:, k * 128:(k + 1) * 128], in_=gated[:, k * 128:(k + 1) * 128], identity=identb[:]),
                         reads=[gated, identb], writes=[pT])
                P.op('act', lambda e: e.activation(out=gT[:].rearrange("p k t -> p (k t)"), in_=pT[:], func=AF.Copy), reads=[pT], writes=[gT])
                xo = x2[ci % 2]
                for cb in range(2):
                    for k in range(8):
                        P.op('pe', lambda e, k=k, cb=cb: e.matmul(P67[:, cb * 512:(cb + 1) * 512], lhsT=gT[:, k, :], rhs=w_o_b[:, k, cb * 512:(cb + 1) * 512],
                                                                 start=(k == 0), stop=(k == 7)), reads=[gT, w_o_b], writes=[P67])
                P.op('dve', lambda e: e.tensor_tensor(out=tmpf[:], in0=P67[:], in1=G1[:], op=ALU.mult), reads=[P67, G1], writes=[tmpf])
                P.op('pool', lambda e: e.tensor_tensor(out=xo[:], in0=tmpf[:], in1=xb[:], op=ALU.add), reads=[tmpf, xb], writes=[xo])
                P.dma('pool', lambda e: e.dma_start(out=X2_d[own_idx * 128:(own_idx + 1) * 128, :], in_=xo[:]), reads=[xo], writes=[X2_d])

        ci = 0
        for c in range(2):
            chunk(c * 128, True, 0, False, False, None, ci); ci += 1
        for c in range(32):
            chunk(LC + c * 128, False, 0, True, False, c, ci); ci += 1
        P.op('pool', lambda e: e.memset(S[:], 0.0), reads=[S_b], writes=[S])
        P.op('pool', lambda e: e.memset(S_b[:], 0.0), reads=[S], writes=[S_b])
        for c in (1, 0):
            chunk(c * 128, True, 1, False, False, None, ci); ci += 1
        for c in range(63, 31, -1):
            chunk(LC + c * 128, False, 1, False, False, None, ci); ci += 1
        for c in range(31, -1, -1):
            chunk(LC + c * 128, False, 1, True, True, c, ci); ci += 1
        fn_r = P.sb("fn_r", [1, D], F32)
        P.dma('sp', lambda e: e.dma_start(out=fn_r[:], in_=fnorm[:, :]), writes=[fn_r])
        for hf in range(2):
            P.op('pe', lambda e, hf=hf: e.matmul(B5[:], lhsT=ones_f[0:1, :], rhs=fn_r[:, hf * 512:(hf + 1) * 512], start=True, stop=True),
                 reads=[ones_f, fn_r], writes=[B5])
            P.op('act', lambda e, hf=hf: e.activation(out=fn_bc[:, hf * 512:(hf + 1) * 512], in_=B5[:], func=AF.Copy), reads=[B5], writes=[fn_bc])
        P.barrier()
        P.stack = base_stack
        sgl.close()
        if stage_limit >= 2:
            with ExitStack() as sf:
                fj = P.sb("fj", [128, D], BF16, sf)
                fs = P.sb("fs", [128, 2], F32, sf)
                fo = [P.sb(f"fo{i}", [128, D], F32, sf) for i in range(2)]

                def writer(P, t, xo):
                    c_ = fs[:, (t % 2):(t % 2) + 1]
                    o_ = fo[t % 2]
                    P.op('act', lambda e: e.activation(out=fj[:], in_=xo[:], func=AF.Square, accum_out=c_), reads=[xo], writes=[fj, fs])
                    P.op('act', lambda e: e.activation(out=c_, in_=c_, func=AF.Sqrt, bias=epsc[:], scale=1.0 / D), reads=[fs, epsc], writes=[fs])
                    P.op('dve', lambda e: e.reciprocal(out=c_, in_=c_), reads=[fs], writes=[fs])
                    P.op('dve', lambda e: e.scalar_tensor_tensor(out=o_[:], in0=xo[:], scalar=c_, in1=fn_bc[:], op0=ALU.mult, op1=ALU.mult),
                         reads=[xo, fs, fn_bc], writes=[o_])
                    P.dma('pool', lambda e: e.dma_start(out=out[t * 128:(t + 1) * 128, :], in_=o_[:]), reads=[o_])

                emit_moe(P, nc, X2_d.t, NT_OWN, lambda t: (A2m, SH2m, G2m), m_wr, m_br, m_wgu, m_bgu, m_wd, m_bd,
                         ident, identb, ones_f, epsc, writer)
        else:
            with ExitStack() as sf:
                cp = [P.sb(f"cp{i}", [128, D], F32, sf) for i in range(2)]
                for t in range(NT_OWN):
                    P.dma('sp', lambda e, t=t: e.dma_start(out=cp[t % 2][:], in_=X2_d[t * 128:(t + 1) * 128, :]), reads=[X2_d], writes=[cp[t % 2]])
                    P.dma('pool', lambda e, t=t: e.dma_start(out=out[t * 128:(t + 1) * 128, :], in_=cp[t % 2][:]), reads=[cp[t % 2]])
        P.finish()
        P.emit()
        print("L1 instr counts", P.cnt, "waits", P.n_wait, "dmas", P.n_dma)
    return nc


def make_in_maps_l1(inp, x1_full, xc_full):
    f = lambda a: np.ascontiguousarray(np.asarray(a, dtype=np.float32))
    c = f(inp['c']); c_ctx = f(inp['c_ctx'])
    tri = np.stack([np.triu(np.ones((128, 128), np.float32)), np.tril(np.ones((128, 128), np.float32))])
    shared = {
        "ident": np.eye(128, dtype=np.float32), "tri": tri,
        "mod_w1": f(inp['mod_w'][1]), "mod_b1": f(inp['mod_b'][1]).reshape(1, 6 * D),
        "norm1_1": f(inp['norm1'][1]).reshape(1, D), "norm2_1": f(inp['norm2'][1]).reshape(1, D),
        "gla_w_in": f(inp['gla_w_in'][0]), "gla_hn": np.ascontiguousarray(np.tile(f(inp['gla_head_norm'][0]), 4).reshape(1, 1024)),
        "gla_w_o": f(inp['gla_w_o'][0]),
        "final_norm": f(inp['final_norm']).reshape(1, D),
        "moe_w_router": f(inp['moe_w_router'][1]), "moe_b_router": f(inp['moe_b_router'][1]).reshape(1, NE),
        "moe_w_gu": f(inp['moe_w_gu'][1]), "moe_b_gu": f(inp['moe_b_gu'][1]),
        "moe_w_down": f(inp['moe_w_down'][1]), "moe_b_down": f(inp['moe_b_down'][1]),
    }
    g1 = f(inp['gla_w_gate1'][0]); g2 = f(inp['gla_w_gate2'][0]); bg = f(inp['gla_b_gate'][0]).reshape(2, 1, 512)
    maps = []
    for core in range(8):
        b, half = core // 2, core % 2
        if half == 0:
            xs = np.concatenate([xc_full[b], x1_full[b]], axis=0)
            sel = [0, 1]
        else:
            xs = np.concatenate([xc_full[b][::-1], x1_full[b][::-1]], axis=0)
            sel = [1, 0]
        m = dict(shared)
        m.update({"xs": np.ascontiguousarray(xs.astype(np.float32)),
                  "cvec": np.ascontiguousarray(np.stack([c[b].reshape(8, 128).T, c_ctx.reshape(8, 128).T], axis=2)),
                  "gla_w_g1": np.ascontiguousarray(g1[sel]), "gla_w_g2": np.ascontiguousarray(g2[sel]), "gla_b_g": np.ascontiguousarray(bg[sel])})
        maps.append(m)
    return maps


NE = 32
SW_ALPHA = 1.702
SW_LIMIT = 7.0


def emit_moe(P, nc, xin, n_tiles, mods, w_router, b_router, w_gu, b_gu, w_down, b_down, ident, identb, ones_f, epsc, writer):
    with ExitStack() as sm:
        wr_f = P.sb("wr_f", [128, 8, NE], F32, sm)
        br_f = P.sb("br_f", [1, NE], F32, sm)
        bd_f = P.sb("bd_f", [NE, D], F32, sm)
        P.dma('sp', lambda e: e.dma_start(out=wr_f[:], in_=w_router.rearrange("(k p) n -> p k n", p=128)), writes=[wr_f])
        P.dma('sp', lambda e: e.dma_start(out=br_f[:], in_=b_router), writes=[br_f])
        P.dma('sp', lambda e: e.dma_start(out=bd_f[:], in_=b_down), writes=[bd_f])
        bgu = P.sb("bgu", [128, NE, 8, 2], F32, sm)
        P.dma('sp', lambda e: e.dma_start(out=bgu[:], in_=b_gu.rearrange("e (c p two) -> p e c two", p=128, two=2)), writes=[bgu])
        xg = P.sb("mxg", [128, 4, D], F32, sm)
        h2f = P.sb("h2f", [128, D], F32, sm)
        h2b = P.sb("h2b", [128, D], BF16, sm)
        junk = h2b
        ssm = P.sb("ssm", [128, 4], F32, sm)
        h2T = P.sb("h2T", [128, 8, 512], BF16, sm)
        h2Tf = P.sb("h2Tf", [128, 8, 128], F32, sm)
        lg = P.sb("lg", [128, NE], F32, sm)
        top8 = P.sb("top8", [128, 8], F32, sm)
        nmax = P.sb("nmax", [128, 1], F32, sm)
        msk = P.sb("msk", [128, NE], F32, sm)
        ex = P.sb("ex", [128, NE], F32, sm)
        rs = P.sb("rs", [128, 1], F32, sm)
        G = P.sb("G", [128, 4, NE], F32, sm)
        GT = P.sb("GT", [NE, 4, 128], F32, sm)
        acc = P.sb("acc", [128, 4, D], F32, sm)
        stg = [P.sb(f"mstg{i}", [128, 1, 2048], F32, sm) for i in range(2)]
        wg = P.sb("wg", [128, 8, D], BF16, sm)
        wu = P.sb("wu", [128, 8, D], BF16, sm)
        wd = P.sb("wd", [128, 8, D], BF16, sm)
        g_sb = [P.sb(f"g_sb{i}", [128, 512], F32, sm) for i in range(2)]
        sg = [P.sb(f"sg{i}", [128, 512], F32, sm) for i in range(2)]
        u_sb = [P.sb(f"u_sb{i}", [128, 512], F32, sm) for i in range(2)]
        actT = P.sb("actT", [128, 8, 512], BF16, sm)
        xn = [P.sb(f"xn{i}", [128, D], F32, sm) for i in range(2)]
        pTb = P.ps("m_pTb", [128, 1024], BF16, sm)
        pTf = P.ps("m_pTf", [128, 1024], F32, sm)
        pl = P.ps("m_pl", [128, 512], F32, sm)
        pg = [P.ps(f"m_pg{i}", [128, 512], F32, sm) for i in range(2)]
        pu = [P.ps(f"m_pu{i}", [128, 512], F32, sm) for i in range(2)]

        n_blocks = (n_tiles + 3) // 4
        for tb in range(n_blocks):
            nt = min(4, n_tiles - tb * 4)
            tw = nt * 128
            tok0 = tb * 512
            P.dma('sp', lambda e, tok0=tok0, nt=nt, tw=tw: e.dma_start(
                out=xg[:, 0:nt, :], in_=xin[tok0:tok0 + tw, :].rearrange("(t p) d -> p t d", p=128)), writes=[xg])
            for tl in range(nt):
                A2, SH2, G2 = mods(tb * 4 + tl)
                xt_ = xg[:, tl, :]
                P.op('act', lambda e, xt_=xt_, tl=tl: e.activation(out=junk[:], in_=xt_, func=AF.Square, accum_out=ssm[:, tl:tl + 1]), reads=[xg], writes=[junk, ssm])
                P.op('act', lambda e, tl=tl: e.activation(out=ssm[:, tl:tl + 1], in_=ssm[:, tl:tl + 1], func=AF.Sqrt, bias=epsc[:], scale=1.0 / D), reads=[ssm, epsc], writes=[ssm])
                P.op('dve', lambda e, tl=tl: e.reciprocal(out=ssm[:, tl:tl + 1], in_=ssm[:, tl:tl + 1]), reads=[ssm], writes=[ssm])
                P.op('dve', lambda e, xt_=xt_, tl=tl, A2=A2: e.scalar_tensor_tensor(out=h2f[:], in0=xt_, scalar=ssm[:, tl:tl + 1], in1=A2[:], op0=ALU.mult, op1=ALU.mult),
                     reads=[xg, ssm, A2], writes=[h2f])
                P.op('pool', lambda e, SH2=SH2: e.tensor_tensor(out=h2f[:], in0=h2f[:], in1=SH2[:], op=ALU.add), reads=[h2f, SH2], writes=[h2f])
                P.op('act', lambda e: e.activation(out=h2b[:], in_=h2f[:], func=AF.Copy), reads=[h2f], writes=[h2b])
                for k in range(8):
                    P.op('pe', lambda e, k=k: e.transpose(out=pTb[:, k * 128:(k + 1) * 128], in_=h2b[:, k * 128:(k + 1) * 128], identity=identb[:]),
                         reads=[h2b, identb], writes=[pTb])
                P.op('act', lambda e, tl=tl: e.activation(out=h2T[:, :, tl * 128:(tl + 1) * 128], in_=pTb[:].rearrange("p (k t) -> p k t", k=8), func=AF.Copy),
                     reads=[pTb], writes=[h2T])
                for k in range(8):
                    P.op('pe', lambda e, k=k: e.transpose(out=pTf[:, k * 128:(k + 1) * 128], in_=h2f[:, k * 128:(k + 1) * 128], identity=ident[:]),
                         reads=[h2f, ident], writes=[pTf])
                P.op('dve', lambda e: e.tensor_copy(out=h2Tf[:].rearrange("p k t -> p (k t)"), in_=pTf[:]), reads=[pTf], writes=[h2Tf])
                for k in range(8):
                    P.op('pe', lambda e, k=k: e.matmul(pl[:, 0:NE], lhsT=h2Tf[:, k, :], rhs=wr_f[:, k, :], start=(k == 0), stop=False),
                         reads=[h2Tf, wr_f], writes=[pl])
                P.op('pe', lambda e: e.matmul(pl[:, 0:NE], lhsT=ones_f[0:1, :], rhs=br_f[:], start=False, stop=True), reads=[ones_f, br_f], writes=[pl])
                P.op('act', lambda e: e.activation(out=lg[:], in_=pl[:, 0:NE], func=AF.Copy), reads=[pl], writes=[lg])
                P.op('dve', lambda e: e.max(out=top8[:], in_=lg[:]), reads=[lg], writes=[top8])
                P.op('dve', lambda e: e.tensor_scalar(out=msk[:], in0=lg[:], scalar1=top8[:, 3:4], scalar2=None, op0=ALU.is_ge), reads=[lg, top8], writes=[msk])
                P.op('dve', lambda e: e.tensor_scalar(out=nmax[:], in0=top8[:, 0:1], scalar1=-1.0, scalar2=None, op0=ALU.mult), reads=[top8], writes=[nmax])
                P.op('act', lambda e: e.activation(out=ex[:], in_=lg[:], func=AF.Exp, bias=nmax[:], scale=1.0), reads=[lg, nmax], writes=[ex])
                P.op('dve', lambda e: e.tensor_tensor(out=ex[:], in0=ex[:], in1=msk[:], op=ALU.mult), reads=[ex, msk], writes=[ex])
                P.op('dve', lambda e: e.tensor_reduce(out=rs[:], in_=ex[:], axis=mybir.AxisListType.X, op=ALU.add), reads=[ex], writes=[rs])
                P.op('dve', lambda e: e.reciprocal(out=rs[:], in_=rs[:]), reads=[rs], writes=[rs])
                P.op('dve', lambda e, tl=tl: e.tensor_scalar(out=G[:, tl, :], in0=ex[:], scalar1=rs[:, 0:1], scalar2=None, op0=ALU.mult), reads=[ex, rs], writes=[G])
                P.op('pe', lambda e, tl=tl: e.transpose(out=pl[0:NE, 128:256], in_=G[:, tl, :], identity=ident[:]), reads=[G, ident], writes=[pl])
                P.op('act', lambda e, tl=tl: e.activation(out=GT[:, tl, :], in_=pl[0:NE, 128:256], func=AF.Copy), reads=[pl], writes=[GT])
                for cb in range(2):
                    P.op('pe', lambda e, tl=tl, cb=cb: e.matmul(pTf[:, cb * 512:(cb + 1) * 512], lhsT=GT[:, tl, :], rhs=bd_f[:, cb * 512:(cb + 1) * 512], start=True, stop=True),
                         reads=[GT, bd_f], writes=[pTf])
                P.op('act', lambda e, tl=tl: e.activation(out=acc[:, tl, :], in_=pTf[:], func=AF.Copy), reads=[pTf], writes=[acc])
            for ex_i in range(NE):
                for qq in range(8):
                    s_ = stg[qq % 2]
                    P.dma('sp' if qq % 2 == 0 else 'pool', lambda e, s_=s_, qq=qq, ex_i=ex_i: e.dma_start(
                        out=s_[:], in_=w_gu[ex_i].rearrange("(k p) n -> p k n", p=128)[:, qq:qq + 1, :]), writes=[s_])
                    sv = s_[:].rearrange("p k (n two) -> p k n two", two=2)
                    P.op('pool', lambda e, sv=sv, qq=qq: e.tensor_copy(out=wg[:, qq:qq + 1, :], in_=sv[:, :, :, 0]), reads=[s_], writes=[wg])
                    P.op('act', lambda e, sv=sv, qq=qq: e.activation(out=wu[:, qq:qq + 1, :], in_=sv[:, :, :, 1], func=AF.Copy), reads=[s_], writes=[wu])
                for hq in range(4):
                    s_ = stg[hq % 2]
                    sd = s_[:].rearrange("p k n -> p (k n)").rearrange("p (k n) -> p k n", k=2)
                    P.dma('sp' if hq % 2 == 0 else 'pool', lambda e, sd=sd, hq=hq, ex_i=ex_i: e.dma_start(
                        out=sd, in_=w_down[ex_i].rearrange("(k p) n -> p k n", p=128)[:, hq * 2:(hq + 1) * 2, :]), writes=[s_])
                    P.op('pool', lambda e, sd=sd, hq=hq: e.tensor_copy(out=wd[:, hq * 2:(hq + 1) * 2, :], in_=sd), reads=[s_], writes=[wd])
                for fc in range(8):
                    i2 = fc % 2
                    for k in range(8):
                        P.op('pe', lambda e, k=k, fc=fc, i2=i2, tw=tw: e.matmul(pg[i2][:, 0:tw], lhsT=wg[:, k, fc * 128:(fc + 1) * 128], rhs=h2T[:, k, 0:tw], start=(k == 0), stop=(k == 7)),
                             reads=[wg, h2T], writes=[pg[i2]])
                    for k in range(8):
                        P.op('pe', lambda e, k=k, fc=fc, i2=i2, tw=tw: e.matmul(pu[i2][:, 0:tw], lhsT=wu[:, k, fc * 128:(fc + 1) * 128], rhs=h2T[:, k, 0:tw], start=(k == 0), stop=(k == 7)),
                             reads=[wu, h2T], writes=[pu[i2]])
                    P.op('dve', lambda e, fc=fc, i2=i2, ex_i=ex_i, tw=tw: e.tensor_scalar(out=g_sb[i2][:, 0:tw], in0=pg[i2][:, 0:tw], scalar1=bgu[:, ex_i, fc, 0:1], scalar2=SW_LIMIT,
                                                                                     op0=ALU.add, op1=ALU.min), reads=[pg[i2], bgu], writes=[g_sb[i2]])
                    P.op('act', lambda e, i2=i2, tw=tw: e.activation(out=sg[i2][:, 0:tw], in_=g_sb[i2][:, 0:tw], func=AF.Sigmoid, scale=SW_ALPHA), reads=[g_sb[i2]], writes=[sg[i2]])
                    P.op('dve', lambda e, fc=fc, i2=i2, ex_i=ex_i, tw=tw: e.tensor_scalar(out=u_sb[i2][:, 0:tw], in0=pu[i2][:, 0:tw], scalar1=bgu[:, ex_i, fc, 1:2], scalar2=SW_LIMIT,
                                                                                     op0=ALU.add, op1=ALU.min), reads=[pu[i2], bgu], writes=[u_sb[i2]])
                    P.op('pool', lambda e, i2=i2, tw=tw: e.tensor_scalar(out=u_sb[i2][:, 0:tw], in0=u_sb[i2][:, 0:tw], scalar1=-SW_LIMIT, scalar2=1.0, op0=ALU.max, op1=ALU.add),
                         reads=[u_sb[i2]], writes=[u_sb[i2]])
                    P.op('pool', lambda e, i2=i2, tw=tw: e.tensor_tensor(out=g_sb[i2][:, 0:tw], in0=g_sb[i2][:, 0:tw], in1=sg[i2][:, 0:tw], op=ALU.mult), reads=[g_sb[i2], sg[i2]], writes=[g_sb[i2]])
                    P.op('dve', lambda e, fc=fc, i2=i2, tw=tw: e.tensor_tensor(out=actT[:, fc, 0:tw], in0=g_sb[i2][:, 0:tw], in1=u_sb[i2][:, 0:tw], op=ALU.mult),
                         reads=[g_sb[i2], u_sb[i2]], writes=[actT])
                for tl in range(nt):
                    for cb in range(2):
                        for fc in range(8):
                            P.op('pe', lambda e, tl=tl, cb=cb, fc=fc: e.matmul(pTf[:, cb * 512:(cb + 1) * 512], lhsT=actT[:, fc, tl * 128:(tl + 1) * 128], rhs=wd[:, fc, cb * 512:(cb + 1) * 512],
                                                                           start=(fc == 0), stop=(fc == 7)), reads=[actT, wd], writes=[pTf])
                    P.op('dve', lambda e, tl=tl, ex_i=ex_i: e.scalar_tensor_tensor(out=acc[:, tl, :], in0=pTf[:], scalar=G[:, tl, ex_i:ex_i + 1], in1=acc[:, tl, :], op0=ALU.mult, op1=ALU.add),
                         reads=[pTf, G, acc], writes=[acc])
            for tl in range(nt):
                t = tb * 4 + tl
                A2, SH2, G2 = mods(t)
                xo = xn[t % 2]
                P.op('dve', lambda e, tl=tl, G2=G2: e.tensor_tensor(out=acc[:, tl, :], in0=acc[:, tl, :], in1=G2[:], op=ALU.mult), reads=[acc, G2], writes=[acc])
                P.op('pool', lambda e, tl=tl, xo=xo: e.tensor_tensor(out=xo[:], in0=acc[:, tl, :], in1=xg[:, tl, :], op=ALU.add), reads=[acc, xg], writes=[xo])
                writer(P, t, xo)
    P.barrier()


def kernel(**inputs):
    nc0 = build_program()
    res0 = run_bass_kernel_spmd(nc0, make_in_maps(inputs), core_ids=list(range(8)))
    x1 = np.zeros((B, N, D), np.float32)
    xc = np.zeros((B, LC, D), np.float32)
    for core in range(8):
        b, half = core // 2, core % 2
        x1[b, half * NOWN:(half + 1) * NOWN] = res0.results[core]["out"]
        if half == 0:
            xc[b] = res0.results[core]["outc"]
    nc1 = build_layer1()
    res1 = run_bass_kernel_spmd(nc1, make_in_maps_l1(inputs, x1, xc), core_ids=list(range(8)))
    outp = np.zeros((B, N, D), np.float32)
    for core in range(8):
        b, half = core // 2, core % 2
        o = res1.results[core]["out"]
        if half == 0:
            outp[b, :NOWN] = o
        else:
            outp[b, NOWN:] = o[::-1]
    return outp
```
